# Optimizing a Trainium2 kernel written in Bass

```python
import math
import jax, jax.numpy as jnp
from jax import lax
import numpy as np


D_MODEL = 1024
BATCH = 4
SEQ = 4096
DEPTH = 2

N_A = DEPTH // 2
N_B = DEPTH - N_A
N_DENSE = (DEPTH + 1) // 2
N_MOE = DEPTH // 2

POOL_WINDOWS = (2, 4, 8, 16)
N_POOL_GROUPS = len(POOL_WINDOWS)
POOL_GW = D_MODEL // N_POOL_GROUPS

N_HEADS = 8
D_NOPE = 128
D_ROPE = 64
D_V = 128
Q_LORA = 384
KV_LORA = 256
ROPE_THETA = 10000.0
Q_BLOCK = 128
ATTN_SCALE = 1.0 / math.sqrt(D_NOPE + D_ROPE)

D_FF = 3584
N_EXPERTS = 8
TOP_K = 2

EPS = 1e-6

kernel_name = "yoco_pool_mla_moe_hybrid"


def rmsnorm(x, g):
    xf = x.astype(jnp.float32)
    y = xf * lax.rsqrt(jnp.mean(xf * xf, axis=-1, keepdims=True) + EPS)
    return y.astype(x.dtype) * g


def modulate(h, shift, scale):
    return h * (1.0 + scale) + shift


def rope(x, cos, sin):
    x1, x2 = jnp.split(x, 2, axis=-1)
    return jnp.concatenate([x1 * cos - x2 * sin, x2 * cos + x1 * sin], axis=-1)


def causal_pool_diff(u, w):
    S = u.shape[1]
    uf = u.astype(jnp.float32)
    cs0 = jnp.pad(jnp.cumsum(uf, axis=1), ((0, 0), (1, 0), (0, 0)))
    upper = cs0[:, 1:]
    lower = jnp.pad(cs0[:, :S - w + 1], ((0, 0), (w - 1, 0), (0, 0)))
    count = jnp.minimum(jnp.arange(1, S + 1), w).astype(jnp.float32)[None, :, None]
    return ((upper - lower) / count - uf).astype(u.dtype)


def pool_mixer(h, w_in, w_grp, scale, w_out):
    B, S, D = h.shape
    u = h @ w_in
    groups = [causal_pool_diff(u[..., g * POOL_GW:(g + 1) * POOL_GW], POOL_WINDOWS[g])
              for g in range(N_POOL_GROUPS)]
    p = jnp.stack(groups, axis=2)
    z = jnp.einsum('bsgc,gcd->bsgd', p, w_grp).reshape(B, S, D) * scale
    return z @ w_out


def shared_kv(x, sc, kv_ada_w, kv_ada_b, kv_norm_g, w_dkv, ckv_norm_g, w_kr, w_uk, w_uv, cos, sin):
    B, S, _ = x.shape
    mod = (sc @ kv_ada_w + kv_ada_b)[:, None, :]
    shift, scale = jnp.split(mod, 2, axis=-1)
    hk = modulate(rmsnorm(x, kv_norm_g), shift, scale)
    c_kv = rmsnorm(hk @ w_dkv, ckv_norm_g)
    k_nope = (c_kv @ w_uk).reshape(B, S, N_HEADS, D_NOPE)
    v = (c_kv @ w_uv).reshape(B, S, N_HEADS, D_V)
    k_rope = rope(hk @ w_kr, cos, sin)
    return k_nope, k_rope, v


def mla_mixer(h, kv, w_dq, q_norm_g, w_uq, w_o, cos, sin):
    B, S, _ = h.shape
    k_nope, k_rope, v = kv
    cq = rmsnorm(h @ w_dq, q_norm_g)
    q = (cq @ w_uq).reshape(B, S, N_HEADS, D_NOPE + D_ROPE)
    q_nope = q[..., :D_NOPE]
    q_rope = rope(q[..., D_NOPE:], cos[:, :, None, :], sin[:, :, None, :])
    nb = S // Q_BLOCK
    qn = q_nope.reshape(B, nb, Q_BLOCK, N_HEADS, D_NOPE).transpose(1, 0, 2, 3, 4)
    qr = q_rope.reshape(B, nb, Q_BLOCK, N_HEADS, D_ROPE).transpose(1, 0, 2, 3, 4)
    key_idx = jnp.arange(S)

    def block(args):
        i, qn_b, qr_b = args
        s = (jnp.einsum('bqhd,bkhd->bhqk', qn_b, k_nope)
             + jnp.einsum('bqhd,bkd->bhqk', qr_b, k_rope)).astype(jnp.float32) * ATTN_SCALE
        q_idx = i * Q_BLOCK + jnp.arange(Q_BLOCK)
        mask = key_idx[None, :] <= q_idx[:, None]
        s = jnp.where(mask[None, None], s, -jnp.inf)
        p = jax.nn.softmax(s, axis=-1).astype(v.dtype)
        return jnp.einsum('bhqk,bkhd->bqhd', p, v)

    o = lax.map(block, (jnp.arange(nb), qn, qr))
    o = o.transpose(1, 0, 2, 3, 4).reshape(B, S, N_HEADS * D_V)
    return o @ w_o


def swiglu(h, w_gate, w_up, w_down):
    return (jax.nn.silu(h @ w_gate) * (h @ w_up)) @ w_down


def moe_swiglu(h, router_w, w_gate, w_up, w_down):
    B, S, D = h.shape
    t = h.reshape(-1, D)
    logits = (t @ router_w).astype(jnp.float32)
    top_v, top_i = lax.top_k(logits, TOP_K)
    top_w = jax.nn.softmax(top_v, axis=-1)
    gates = jnp.sum(jax.nn.one_hot(top_i, N_EXPERTS, dtype=jnp.float32) * top_w[..., None], axis=1)
    gates = gates.astype(t.dtype)
    out = jnp.zeros_like(t)
    for e in range(N_EXPERTS):
        out = out + gates[:, e:e + 1] * swiglu(t, w_gate[e], w_up[e], w_down[e])
    return out.reshape(B, S, D)


def setup_inputs(seed: int = 0) -> dict:
    key = jax.random.key(seed)
    ks = iter(jax.random.split(key, 48))
    f32 = jnp.float32
    D = D_MODEL

    def w(shape, fan_in, s=1.0):
        return jax.random.normal(next(ks), shape, f32) * (s * fan_in ** -0.5)

    def gain(shape):
        return 1.0 + 0.05 * jax.random.normal(next(ks), shape, f32)

    def bias(shape):
        return 0.01 * jax.random.normal(next(ks), shape, f32)

    x = jax.random.normal(next(ks), (BATCH, SEQ, D), f32)
    c = jax.random.normal(next(ks), (BATCH, D), f32)
    offsets = jax.random.randint(next(ks), (BATCH, 1), 0, 1024, dtype=jnp.int32)
    positions = offsets + jnp.arange(SEQ, dtype=jnp.int32)[None, :]
    return {
        "x": x,
        "c": c,
        "positions": positions,
        "ada_w": w((DEPTH, D, 6 * D), D, 0.5),
        "ada_b": bias((DEPTH, 6 * D)),
        "mix_norm_g": gain((DEPTH, D)),
        "ffn_norm_g": gain((DEPTH, D)),
        "pool_w_in": w((N_A, D, D), D),
        "pool_w_grp": w((N_A, N_POOL_GROUPS, POOL_GW, POOL_GW), POOL_GW),
        "pool_scale": 1.0 + 0.1 * jax.random.normal(next(ks), (N_A, D), f32),
        "pool_w_out": w((N_A, D, D), D),
        "kv_ada_w": w((D, 2 * D), D, 0.5),
        "kv_ada_b": bias((2 * D,)),
        "kv_norm_g": gain((D,)),
        "w_dkv": w((D, KV_LORA), D),
        "ckv_norm_g": gain((KV_LORA,)),
        "w_kr": w((D, D_ROPE), D),
        "w_uk": w((KV_LORA, N_HEADS * D_NOPE), KV_LORA),
        "w_uv": w((KV_LORA, N_HEADS * D_V), KV_LORA),
        "w_dq": w((N_B, D, Q_LORA), D),
        "q_norm_g": gain((N_B, Q_LORA)),
        "w_uq": w((N_B, Q_LORA, N_HEADS * (D_NOPE + D_ROPE)), Q_LORA),
        "w_o": w((N_B, N_HEADS * D_V, D), N_HEADS * D_V),
        "ffn_w_gate": w((N_DENSE, D, D_FF), D),
        "ffn_w_up": w((N_DENSE, D, D_FF), D),
        "ffn_w_down": w((N_DENSE, D_FF, D), D_FF),
        "router_w": w((N_MOE, D, N_EXPERTS), D),
        "moe_w_gate": w((N_MOE, N_EXPERTS, D, D_FF), D),
        "moe_w_up": w((N_MOE, N_EXPERTS, D, D_FF), D),
        "moe_w_down": w((N_MOE, N_EXPERTS, D_FF, D), D_FF),
        "final_norm_g": gain((D,)),
    }


def reference(x, c, positions, ada_w, ada_b, mix_norm_g, ffn_norm_g, pool_w_in, pool_w_grp,
              pool_scale, pool_w_out, kv_ada_w, kv_ada_b, kv_norm_g, w_dkv, ckv_norm_g, w_kr,
              w_uk, w_uv, w_dq, q_norm_g, w_uq, w_o, ffn_w_gate, ffn_w_up, ffn_w_down,
              router_w, moe_w_gate, moe_w_up, moe_w_down, final_norm_g):
    sc = jax.nn.silu(c)
    inv_freq = ROPE_THETA ** (-jnp.arange(0, D_ROPE, 2, dtype=jnp.float32) / D_ROPE)
    ang = positions.astype(jnp.float32)[..., None] * inv_freq
    cos = jnp.cos(ang).astype(x.dtype)
    sin = jnp.sin(ang).astype(x.dtype)
    kv = None
    for layer in range(DEPTH):
        mod = (sc @ ada_w[layer] + ada_b[layer])[:, None, :]
        sh1, sc1, g1, sh2, sc2, g2 = jnp.split(mod, 6, axis=-1)
        h = modulate(rmsnorm(x, mix_norm_g[layer]), sh1, sc1)
        if layer < N_A:
            y = pool_mixer(h, pool_w_in[layer], pool_w_grp[layer], pool_scale[layer], pool_w_out[layer])
        else:
            if layer == N_A:
                kv = shared_kv(x, sc, kv_ada_w, kv_ada_b, kv_norm_g, w_dkv, ckv_norm_g,
                               w_kr, w_uk, w_uv, cos, sin)
            j = layer - N_A
            y = mla_mixer(h, kv, w_dq[j], q_norm_g[j], w_uq[j], w_o[j], cos, sin)
        x = x + g1 * y
        h = modulate(rmsnorm(x, ffn_norm_g[layer]), sh2, sc2)
        if layer % 2 == 0:
            i = layer // 2
            y = swiglu(h, ffn_w_gate[i], ffn_w_up[i], ffn_w_down[i])
        else:
            i = layer // 2
            y = moe_swiglu(h, router_w[i], moe_w_gate[i], moe_w_up[i], moe_w_down[i])
        x = x + g2 * y
    return rmsnorm(x, final_norm_g)
```

```python
import contextlib
import numpy as np
import ml_dtypes
import concourse.bass as bass
import concourse.mybir as mybir
from concourse.bass_utils import run_bass_kernel_spmd

F32 = mybir.dt.float32
BF16 = mybir.dt.bfloat16
I32 = mybir.dt.int32
ALU = mybir.AluOpType
AF = mybir.ActivationFunctionType
AX = mybir.AxisListType

ENGS = ("pe", "act", "dve", "pool", "sp")

D = 1024
T = 2048
NT = 16
NG = 4
GW = 512
DFF = 3584
NF = 28
FB = 4
NFB = NF // FB
NE = 8
EPS = 1e-6
ATTN_SCALE = 1.0 / float(np.sqrt(192.0))
PI = float(np.pi)


class Res:
    __slots__ = ("w", "r", "name")

    def __init__(self, name=""):
        self.w = None
        self.r = {}
        self.name = name


class Sched:
    def __init__(self, nc, stack, n_dma=32, self_wait=True):
        self.nc = nc
        self.ops = {e: [] for e in ENGS}
        self.cnt = {e: 0 for e in ENGS}
        self.seen = {e: {} for e in ENGS}
        self.sems = {}
        for e in ("pe", "act", "dve", "pool"):
            self.sems[e] = stack.enter_context(nc.semaphore("s_" + e))
        self.n_dma = n_dma
        for i in range(n_dma):
            self.sems[("d", i)] = stack.enter_context(nc.semaphore("s_d%d" % i))
        self.dma_cnt = [0] * n_dma
        self.dma_rr = 0
        self.self_wait = self_wait
        self.out_ticks = []

    def _waits(self, eng, reads, writes, extra=()):
        waits = {}

        def need(t):
            if t is None:
                return
            k, v = t
            if k == eng and (eng == "pe" or not self.self_wait):
                return
            if waits.get(k, 0) < v:
                waits[k] = v

        for r in reads:
            need(r.w)
        for w in writes:
            if w.w is not None and w.w[0] != eng:
                need(w.w)
            for k, v in w.r.items():
                if k != eng:
                    need((k, v))
        for t in extra:
            need(t)
        seen = self.seen[eng]
        wl = []
        for k, v in waits.items():
            if seen.get(k, 0) >= v:
                continue
            seen[k] = v
            wl.append((k, v))
        return wl

    def op(self, eng, fn, reads=(), writes=(), inc=True):
        wl = self._waits(eng, reads, writes)
        if inc:
            self.cnt[eng] += 1
            tick = (eng, self.cnt[eng])
        else:
            tick = (eng, self.cnt[eng] + 1)
        self.ops[eng].append((fn, wl, eng if inc else None))
        for r in reads:
            if r.r.get(eng, 0) < tick[1]:
                r.r[eng] = tick[1]
        for w in writes:
            w.w = tick
            w.r = {}
        return tick

    def dma(self, q, out, in_, reads=(), writes=(), **kw):
        i = self.dma_rr
        self.dma_rr = (i + 1) % self.n_dma
        key = ("d", i)
        prev = (key, 16 * self.dma_cnt[i]) if self.dma_cnt[i] else None
        wl = self._waits(q, reads, writes, extra=(prev,) if prev else ())
        self.dma_cnt[i] += 1
        tick = (key, 16 * self.dma_cnt[i])

        def fn(e, out=out, in_=in_, kw=kw):
            return e.dma_start(out=out, in_=in_, **kw)

        self.ops[q].append((fn, wl, key))
        for r in reads:
            if r.r.get(key, 0) < tick[1]:
                r.r[key] = tick[1]
        for w in writes:
            w.w = tick
            w.r = {}
        return tick

    def raw(self, q, fn, key_inc=None, reads=(), writes=(), extra=()):
        i = self.dma_rr
        self.dma_rr = (i + 1) % self.n_dma
        key = ("d", i)
        prev = (key, 16 * self.dma_cnt[i]) if self.dma_cnt[i] else None
        wl = self._waits(q, reads, writes, extra=tuple(extra) + ((prev,) if prev else ()))
        self.dma_cnt[i] += 1
        tick = (key, 16 * self.dma_cnt[i])
        self.ops[q].append((fn, wl, key))
        for r in reads:
            if r.r.get(key, 0) < tick[1]:
                r.r[key] = tick[1]
        for w in writes:
            w.w = tick
            w.r = {}
        return tick

    def wait_all(self, eng, tickets):
        wl = self._waits(eng, (), (), extra=tickets)
        self.ops[eng].append((None, wl, None))

    def flush(self):
        allt = [(("d", i), 16 * c) for i, c in enumerate(self.dma_cnt) if c]
        self.wait_all("sp", allt)
        fin = [(k, self.cnt[k]) for k in ("pe", "act", "dve", "pool") if self.cnt[k]]
        for eng in ENGS:
            self.wait_all(eng, [t for t in fin if t[0] != eng])
        nc = self.nc
        handles = {"pe": "tensor", "act": "scalar", "dve": "vector", "pool": "gpsimd", "sp": "sync"}
        with nc.Block() as block:
            for ename in ENGS:
                ops = self.ops[ename]
                if not ops:
                    continue

                def body(e, ops=ops):
                    for fn, wl, inc in ops:
                        for k, v in wl:
                            e.wait_ge(self.sems[k], v)
                        if fn is None:
                            continue
                        ins = fn(e)
                        if inc is not None:
                            ins.then_inc(self.sems[inc], 16 if isinstance(inc, tuple) else 1)

                getattr(block, handles[ename])(body)
        self.ops = {e: [] for e in ENGS}


def run_pipelined(gens, depth=2):
    it = iter(gens)
    active = []
    exhausted = False
    while True:
        for g in list(active):
            try:
                next(g)
            except StopIteration:
                active.remove(g)
        if not exhausted and len(active) < depth:
            try:
                g = next(it)
                active.append(g)
                next(g)
            except StopIteration:
                exhausted = True
        if exhausted and not active:
            break


VEC_MIXG = (0, 8)
VEC_FFNG = 16
VEC_KVG = 32
VEC_PSC = 40
VEC_FING = 48
VEC_CKVG = 56
VEC_QG = 58
NVEC = 64


def build(mode="AB", dbg=()):
    nc = bass.Bass("TRN2", target_bir_lowering=False)
    doA = True
    doB = True

    def din(name, shape, dt=F32):
        return nc.dram_tensor(name, list(shape), dt, kind="ExternalInput").ap()

    def dout(name, shape, dt=F32):
        return nc.dram_tensor(name, list(shape), dt, kind="ExternalOutput").ap()

    I = {}
    for sfx in ("", "_p"):
        I["xT" + sfx] = din("xT" + sfx, [D, T])
        I["xhT" + sfx] = din("xhT" + sfx, [D, 256])
        I["hmask" + sfx] = din("hmask" + sfx, [128, 256])
        I["invc" + sfx] = din("invc" + sfx, [128, 8, 16])
        I["pos" + sfx] = din("pos" + sfx, [1, T], I32)
    I["cT"] = din("cT", [128, 8])
    I["vecs"] = din("vecs", [128, NVEC])
    I["ident"] = din("ident", [128, 128])
    I["ada_w"] = din("ada_w", [2, D, 6 * D])
    I["ada_b"] = din("ada_b", [1, 12 * D])
    I["pool_w_in"] = din("pool_w_in", [D, D])
    I["pool_w_grp"] = din("pool_w_grp", [4, 256, 256])
    I["pool_w_out"] = din("pool_w_out", [D, D])
    I["ffn_w_gate"] = din("ffn_w_gate", [1, D, DFF])
    I["ffn_w_up"] = din("ffn_w_up", [1, D, DFF])
    I["ffn_w_down"] = din("ffn_w_down", [1, DFF, D])
    I["kv_ada_w"] = din("kv_ada_w", [D, 2 * D])
    I["kv_ada_b"] = din("kv_ada_b", [1, 2 * D])
    I["w_dkv"] = din("w_dkv", [D, 256])
    I["w_kr2"] = din("w_kr2", [D, 128])
    I["ropec"] = din("ropec", [64, 2])
    I["amask"] = din("amask", [128, 4, 128])
    I["esel"] = din("esel", [8, 8, 128])
    I["w_uk"] = din("w_uk", [256, D])
    I["w_uv"] = din("w_uv", [256, D])
    I["w_dq"] = din("w_dq", [D, 384])
    I["w_uq_n"] = din("w_uq_n", [384, D])
    I["w_uq_r"] = din("w_uq_r", [384, D])
    I["w_o"] = din("w_o", [D, D])
    I["router_w"] = din("router_w", [D, 8])
    I["moe_w_gate"] = din("moe_w_gate", [NE, D, DFF])
    I["moe_w_up"] = din("moe_w_up", [NE, D, DFF])
    I["moe_w_down"] = din("moe_w_down", [NE, DFF, D])
    O = {}
    O["outT"] = dout("outT", [D, T])
    dbg_out = {}

    with contextlib.ExitStack() as st:
        S = Sched(nc, st)

        _uid = [0]

        def sbuf(stack, name, shape, dt):
            _uid[0] += 1
            return stack.enter_context(nc.sbuf_tensor("sb%d_%s" % (_uid[0], name), list(shape), dt))

        pb = [st.enter_context(nc.psum_tensor("pb%d" % i, [128, 512], F32)) for i in range(8)]
        pbr = [Res("pb%d" % i) for i in range(8)]
        x = sbuf(st, "x", [128, 8, T], F32)
        xr = [Res("x%d" % g) for g in range(NG)]
        vecs = sbuf(st, "vecs", [128, NVEC], F32)
        modc = sbuf(st, "modc", [128, 112], F32)
        AB = sbuf(st, "ABv", [128, 64], F32)
        ident = sbuf(st, "ident", [128, 128], F32)
        identb = sbuf(st, "identb", [128, 128], BF16)
        ones = sbuf(st, "ones", [128, 128], BF16)
        onesf = sbuf(st, "onesf", [128, 128], F32)
        epsD = sbuf(st, "epsD", [128, 1], F32)
        cres = Res("consts")
        mres = Res("modc")

        S.dma("sp", vecs[:], I["vecs"], writes=[cres])
        S.dma("sp", ident[:], I["ident"], writes=[cres])
        S.dma("pool", identb[:], I["ident"], writes=[cres])
        S.op("dve", lambda e: e.memset(ones[:], 1.0), writes=[cres])
        S.op("dve", lambda e: e.memset(onesf[:], 1.0), writes=[cres])
        S.op("dve", lambda e: e.memset(epsD[:], EPS), writes=[cres])
        Lctx = st.enter_context(contextlib.ExitStack())
        Lc = sbuf(Lctx, "Lc", [128, 2, 2 * T], BF16)
        Lr = sbuf(Lctx, "Lr", [64, 2 * T], BF16)
        Lres = Res("L")
        Lc5 = Lc[:].rearrange("p c (i s q) -> p c i s q", s=2, q=128)
        Lr4 = Lr[:].rearrange("p (i s q) -> p i s q", s=2, q=128)

        def dump(name, ap, shape, dt, res):
            o = dout("dbg_" + name, shape, dt)
            dbg_out[name] = o
            S.dma("sp", o, ap, reads=res)

        class NormScratch:
            def __init__(self, stack, tag, nch=8, n=GW, tmp=None, r_tmp=None):
                self.sq = sbuf(stack, "sq" + tag, [128, nch, n], BF16)
                self.tmp = tmp if tmp is not None else sbuf(stack, "tmp" + tag, [128, nch, n], F32)
                self.rstd = sbuf(stack, "rstd" + tag, [128, n], F32)
                self.r_sq = Res("sq" + tag)
                self.r_tmp = r_tmp if r_tmp is not None else [Res("tmp%s_%d" % (tag, c)) for c in range(nch)]
                self.r_rstd = Res("rstd" + tag)

        def norm_mod(ns, src, src_res, nch, n, dfeat, A, B, out, out_res, bank, extra_reads=(), src_all=None):
            if src_all is not None:
                S.op("act", lambda e: e.activation(ns.sq[:, :, :n], src_all, AF.Square),
                     reads=list(src_res) + list(extra_reads), writes=[ns.r_sq])
            for c in range(nch if src_all is None else 0):
                S.op("act", lambda e, c=c: e.activation(ns.sq[:, c, :n], src(c), AF.Square),
                     reads=list(src_res) + list(extra_reads), writes=[ns.r_sq])
            for c in range(nch):
                S.op("pe", lambda e, c=c: e.matmul(pb[bank][:, :n], ones[:], ns.sq[:, c, :n], start=(c == 0), stop=(c == nch - 1)),
                     reads=[ns.r_sq, cres], writes=[pbr[bank]], inc=(c == nch - 1))
            S.op("act", lambda e: e.activation(ns.rstd[:, :n], pb[bank][:, :n], AF.Sqrt, bias=epsD[:, 0:1], scale=1.0 / dfeat),
                 reads=[pbr[bank], cres], writes=[ns.r_rstd])
            S.op("dve", lambda e: e.reciprocal(ns.rstd[:, :n], ns.rstd[:, :n]), reads=[ns.r_rstd], writes=[ns.r_rstd])
            for c in range(nch):
                if B is None:
                    S.op("dve", lambda e, c=c: e.scalar_tensor_tensor(out(c), src(c), A(c), ns.rstd[:, :n], ALU.mult, ALU.mult),
                         reads=list(src_res) + [ns.r_rstd, mres], writes=list(out_res))
                else:
                    S.op("dve", lambda e, c=c: e.scalar_tensor_tensor(ns.tmp[:, c, :n], src(c), A(c), ns.rstd[:, :n], ALU.mult, ALU.mult),
                         reads=list(src_res) + [ns.r_rstd, mres], writes=[ns.r_tmp[c]])
                    S.op("act", lambda e, c=c: e.activation(out(c), ns.tmp[:, c, :n], AF.Identity, bias=B(c), scale=1.0),
                         reads=[ns.r_tmp[c], mres], writes=list(out_res))

        def col(t, j):
            return t[:, j:j + 1]

        with contextlib.ExitStack() as ph:
            sc = sbuf(ph, "sc", [128, 8], F32)
            scr = Res("sc")
            S.dma("sp", sc[:], I["cT"], writes=[scr])
            S.op("act", lambda e: e.activation(sc[:], sc[:], AF.Silu), reads=[scr], writes=[scr])
            blocks = [("ada", l, j) for l in range(2) for j in range(12)]
            if doA:
                blocks += [("kv", 0, j) for j in range(4)]
            wa = [sbuf(ph, "wa%d" % i, [128, 8, 512], BF16) for i in range(4)]
            scb = sbuf(ph, "scb", [128, 8], BF16)
            S.op("act", lambda e: e.copy(scb[:], sc[:]), reads=[scr], writes=[scr])
            war = [Res("wa%d" % i) for i in range(4)]
            brow = [sbuf(ph, "brow%d" % i, [1, 512], F32) for i in range(4)]
            mrow = [sbuf(ph, "mrow%d" % i, [1, 512], F32) for i in range(2)]
            mrowr = [Res("mrow%d" % i) for i in range(2)]
            ncol = 112 if doA else 96
            for bi, (kind, l, j) in enumerate(blocks):
                s = bi % 4
                if kind == "ada":
                    src = I["ada_w"][l][:, j * 512:(j + 1) * 512].rearrange("(c p) f -> p c f", p=128)
                    off = l * 6 * D + j * 512
                    bsrc = I["ada_b"][:, off:off + 512]
                else:
                    src = I["kv_ada_w"][:, j * 512:(j + 1) * 512].rearrange("(c p) f -> p c f", p=128)
                    off = 12 * D + j * 512
                    bsrc = I["kv_ada_b"][:, j * 512:(j + 1) * 512]
                S.dma("pool", wa[s][:], src, writes=[war[s]])
                S.dma("sp", brow[s][:], bsrc, writes=[war[s]])
                bank = bi % 2
                for c in range(8):
                    S.op("pe", lambda e, c=c, s=s, bank=bank: e.matmul(pb[bank][0:1, :], scb[:, c:c + 1], wa[s][:, c, :], start=(c == 0), stop=(c == 7)),
                         reads=[scr, war[s]], writes=[pbr[bank]], inc=(c == 7))
                S.op("dve", lambda e, s=s, bank=bank: e.tensor_tensor(mrow[bank][0:1, :], pb[bank][0:1, :], brow[s][0:1, :], ALU.add),
                     reads=[pbr[bank], war[s]], writes=[mrowr[bank]])
                for q in range(4):
                    jj = off // 128 + q
                    S.op("pe", lambda e, jj=jj, q=q, bank=bank: e.matmul(pb[2][:, jj:jj + 1], mrow[bank][0:1, q * 128:(q + 1) * 128], onesf[0:1, 0:1], start=True, stop=True),
                         reads=[mrowr[bank], cres], writes=[pbr[2]], inc=(q == 3))
            S.op("dve", lambda e: e.tensor_copy(modc[:, :ncol], pb[2][:, :ncol]), reads=[pbr[2]], writes=[mres])
            for l in range(2):
                S.op("dve", lambda e, l=l: e.scalar_tensor_tensor(AB[:, l * 16:l * 16 + 8], modc[:, l * 48 + 8:l * 48 + 16], 1.0,
                                                                   vecs[:, VEC_MIXG[l]:VEC_MIXG[l] + 8], ALU.add, ALU.mult),
                     reads=[mres, cres], writes=[mres])
                S.op("dve", lambda e, l=l: e.scalar_tensor_tensor(AB[:, l * 16 + 8:l * 16 + 16], modc[:, l * 48 + 32:l * 48 + 40], 1.0,
                                                                   vecs[:, VEC_FFNG + l * 8:VEC_FFNG + l * 8 + 8], ALU.add, ALU.mult),
                     reads=[mres, cres], writes=[mres])
            if doA:
                S.op("dve", lambda e: e.scalar_tensor_tensor(AB[:, 32:40], modc[:, 104:112], 1.0, vecs[:, VEC_KVG:VEC_KVG + 8], ALU.add, ALU.mult),
                     reads=[mres, cres], writes=[mres])
            if "modc" in dbg:
                dump("modc", modc[:], [128, 112], F32, [mres])
            S.flush()

        def A1(l):
            return lambda c: col(AB, l * 16 + c)

        def B1(l):
            return lambda c: col(modc, l * 48 + c)

        def G1(l):
            return lambda c: col(modc, l * 48 + 16 + c)

        def A2(l):
            return lambda c: col(AB, l * 16 + 8 + c)

        def B2(l):
            return lambda c: col(modc, l * 48 + 24 + c)

        def G2(l):
            return lambda c: col(modc, l * 48 + 40 + c)

        def make_tables(ctx, sfx):
            cs_t = sbuf(ctx, "cs_t", [64, T], F32)
            sn_t = sbuf(ctx, "sn_t", [64, T], F32)
            ropr = Res("rope")
            with contextlib.ExitStack() as ph:
                posi = sbuf(ph, "posi", [64, T], I32)
                ang = sbuf(ph, "ang", [64, T], F32)
                r1 = sbuf(ph, "r1", [64, T], F32)
                ropec = sbuf(ph, "ropec", [64, 2], F32)
                tr = Res("ropetmp")
                S.dma("sp", posi[:], I["pos" + sfx].partition_broadcast(64), writes=[tr])
                S.dma("sp", ropec[:], I["ropec"], writes=[tr])
                S.op("dve", lambda e: e.tensor_copy(ang[:], posi[:]), reads=[tr], writes=[tr])
                S.op("dve", lambda e: e.tensor_scalar(ang[:], ang[:], ropec[:, 0:1], None, ALU.mult), reads=[tr], writes=[tr])
                ki = sbuf(ph, "ki", [64, T], I32)
                kf = sbuf(ph, "kf", [64, T], F32)

                def reduce_to_pi(shift):
                    S.op("dve", lambda e: e.tensor_scalar(r1[:], ang[:], shift, None, ALU.add), reads=[tr, ropr], writes=[tr])
                    S.op("dve", lambda e: e.tensor_scalar(kf[:], r1[:], 1.0 / (2 * PI), None, ALU.mult), reads=[tr], writes=[tr])
                    S.op("dve", lambda e: e.tensor_copy(ki[:], kf[:]), reads=[tr], writes=[tr])
                    S.op("dve", lambda e: e.tensor_copy(kf[:], ki[:]), reads=[tr], writes=[tr])
                    S.op("dve", lambda e: e.scalar_tensor_tensor(r1[:], kf[:], -2 * PI, r1[:], ALU.mult, ALU.add), reads=[tr], writes=[tr])
                    S.op("dve", lambda e: e.tensor_scalar(kf[:], r1[:], PI, 2 * PI, ALU.is_gt, ALU.mult), reads=[tr], writes=[tr])
                    S.op("dve", lambda e: e.tensor_tensor(r1[:], r1[:], kf[:], ALU.subtract), reads=[tr], writes=[tr])
                    S.op("dve", lambda e: e.tensor_scalar(kf[:], r1[:], -PI, 2 * PI, ALU.is_lt, ALU.mult), reads=[tr], writes=[tr])
                    S.op("dve", lambda e: e.tensor_tensor(r1[:], r1[:], kf[:], ALU.add), reads=[tr], writes=[tr])
                    S.op("dve", lambda e: e.tensor_scalar(r1[:], r1[:], -3.1415925, 3.1415925, ALU.max, ALU.min), reads=[tr], writes=[tr])

                reduce_to_pi(0.0)
                S.op("act", lambda e: e.activation(sn_t[:], r1[:], AF.Sin, scale=ropec[:, 1:2]), reads=[tr], writes=[ropr])
                reduce_to_pi(PI / 2)
                S.op("act", lambda e: e.activation(cs_t[:], r1[:], AF.Sin), reads=[tr], writes=[ropr])
                if "rope" in dbg:
                    dump("cs", cs_t[:], [64, T], F32, [ropr])
                    dump("sn", sn_t[:], [64, T], F32, [ropr])
                S.flush()
            return cs_t, sn_t, ropr

        def ffn_phase(ph, h2, h2r, wg_src, wu_src, wd_src, n_exp, G, gate_fn):
            wgt = [sbuf(ph, "wg%d" % i, [128, 8, FB * 128], BF16) for i in range(2)]
            wut = [sbuf(ph, "wu%d" % i, [128, 8, FB * 128], BF16) for i in range(2)]
            wdt = [sbuf(ph, "wd%d" % i, [128, FB, D], BF16) for i in range(2)]
            wr = [Res("wslot%d" % i) for i in range(2)]
            ablk = sbuf(ph, "ablk", [128, FB, T], BF16)
            ar = [Res("ablk%d" % g) for g in range(NG)]
            sil = [sbuf(ph, "sil%d" % i, [128, GW], BF16) for i in range(4)]
            silr = [Res("sil%d" % i) for i in range(4)]
            a1 = [sbuf(ph, "a1_%d" % i, [128, GW], BF16) for i in range(4)]
            a1r = [Res("a1_%d" % i) for i in range(4)]
            steps = [(e, fb) for e in range(n_exp) for fb in range(NFB)]

            def load(si):
                e, fb = steps[si]
                s = si % 2
                S.dma("pool", wgt[s][:], wg_src(e, fb), writes=[wr[s]])
                S.dma("pool", wut[s][:], wu_src(e, fb), writes=[wr[s]])
                S.dma("pool", wdt[s][:], wd_src(e, fb), writes=[wr[s]])

            load(0)
            cnt = 0
            dcnt = [0]
            for si, (e, fb) in enumerate(steps):
                s = si % 2
                if si + 1 < len(steps):
                    load(si + 1)
                gw = gate_fn(e) if (gate_fn is not None and fb == 0) else (gate_fn.cur if gate_fn is not None else None)
                for g in range(NG):
                    for fi in range(FB):
                        k = cnt % 2
                        cnt += 1
                        bg, bu = k, 2 + k
                        for c in range(8):
                            S.op("pe", lambda e_, c=c, s=s, fi=fi, g=g, bg=bg: e_.matmul(
                                pb[bg][:], wgt[s][:, c, fi * 128:(fi + 1) * 128], h2[:, c, g * GW:(g + 1) * GW], start=(c == 0), stop=(c == 7)),
                                reads=[wr[s], h2r[g]], writes=[pbr[bg]], inc=(c == 7))
                        for c in range(8):
                            S.op("pe", lambda e_, c=c, s=s, fi=fi, g=g, bu=bu: e_.matmul(
                                pb[bu][:], wut[s][:, c, fi * 128:(fi + 1) * 128], h2[:, c, g * GW:(g + 1) * GW], start=(c == 0), stop=(c == 7)),
                                reads=[wr[s], h2r[g]], writes=[pbr[bu]], inc=(c == 7))
                        kk = (cnt - 1) % 4
                        S.op("act", lambda e_, kk=kk, bg=bg: e_.activation(sil[kk][:], pb[bg][:], AF.Silu),
                             reads=[pbr[bg]], writes=[silr[kk]])
                        if gw is None:
                            S.op("dve", lambda e_, kk=kk, bu=bu, fi=fi, g=g: e_.tensor_tensor(
                                ablk[:, fi, g * GW:(g + 1) * GW], sil[kk][:], pb[bu][:], ALU.mult),
                                reads=[silr[kk], pbr[bu]], writes=[ar[g]])
                        else:
                            gwt, gwr = gw
                            S.op("dve", lambda e_, kk=kk, bu=bu: e_.tensor_tensor(a1[kk][:], sil[kk][:], pb[bu][:], ALU.mult),
                                 reads=[silr[kk], pbr[bu]], writes=[a1r[kk]])
                            S.op("dve", lambda e_, kk=kk, fi=fi, g=g, gwt=gwt: e_.tensor_tensor(
                                ablk[:, fi, g * GW:(g + 1) * GW], a1[kk][:], gwt[:, g * GW:(g + 1) * GW], ALU.mult),
                                reads=[a1r[kk], gwr], writes=[ar[g]])
                for g in range(NG):
                    for dc in range(8):
                        bd = 4 + (dcnt[0] % 3)
                        dcnt[0] += 1
                        for fi in range(FB):
                            S.op("pe", lambda e_, fi=fi, s=s, dc=dc, g=g, bd=bd: e_.matmul(
                                pb[bd][:], wdt[s][:, fi, dc * 128:(dc + 1) * 128], ablk[:, fi, g * GW:(g + 1) * GW], start=(fi == 0), stop=(fi == FB - 1)),
                                reads=[wr[s], ar[g]], writes=[pbr[bd]], inc=(fi == FB - 1))
                        S.op("dve", lambda e_, dc=dc, g=g, bd=bd: e_.scalar_tensor_tensor(
                            x[:, dc, g * GW:(g + 1) * GW], pb[bd][:], G(dc), x[:, dc, g * GW:(g + 1) * GW], ALU.mult, ALU.add),
                            reads=[pbr[bd], mres, xr[g]], writes=[xr[g]])

        def layer0_pass(sfx, slot, tctx):
            xsrc = I["xT" + sfx].rearrange("(c p) t -> p c t", p=128)
            for g in range(NG):
                S.dma("sp", x[:, :, g * GW:(g + 1) * GW], xsrc[:, :, g * GW:(g + 1) * GW], writes=[xr[g]])
            with contextlib.ExitStack() as ph:
                w_in = sbuf(ph, "w_in", [128, 8, D], BF16)
                w_grp = sbuf(ph, "w_grp", [128, 4, 2, 256], BF16)
                w_out = sbuf(ph, "w_out", [128, 8, D], BF16)
                wres = Res("poolw")
                S.dma("pool", w_in[:], I["pool_w_in"].rearrange("(c p) f -> p c f", p=128), writes=[wres])
                S.dma("pool", w_grp[:], I["pool_w_grp"].rearrange("g (k p) f -> p g k f", p=128), writes=[wres])
                S.dma("pool", w_out[:], I["pool_w_out"].rearrange("(c p) f -> p c f", p=128), writes=[wres])
                xh = sbuf(ph, "xh", [128, 8, 256], F32)
                xhr = Res("xh")
                S.dma("sp", xh[:], I["xhT" + sfx].rearrange("(c p) t -> p c t", p=128), writes=[xhr])
                hmask = sbuf(ph, "hmask", [128, 256], F32)
                invc = sbuf(ph, "invc", [128, 8, 16], F32)
                S.dma("sp", hmask[:], I["hmask" + sfx], writes=[cres])
                S.dma("sp", invc[:], I["invc" + sfx], writes=[cres])
                UW = 256
                uh = sbuf(ph, "uh", [128, 8, 16, 16], F32)
                uhr = Res("uh")
                t16 = sbuf(ph, "t16", [128, 2, 16], F32)
                t16r = Res("t16")
                sets = []
                for bs in range(2):
                    big = sbuf(ph, "big%d" % bs, [128, 8, 288], F32)
                    ugr = [Res("ug%d_%d" % (bs, c)) for c in range(8)]
                    d_ = dict(
                        big=big, ugr=ugr, ug=big[:].rearrange("p c (t q) -> p c t q", q=144),
                        ns=NormScratch(ph, "p%d" % bs, n=UW, tmp=big, r_tmp=ugr),
                        h=sbuf(ph, "h_p%d" % bs, [128, 8, UW], BF16), hr=Res("h_p%d" % bs),
                        sA=sbuf(ph, "sA%d" % bs, [128, 2, 2, 144], F32), sAr=Res("sA%d" % bs),
                        sB=sbuf(ph, "sB%d" % bs, [128, 2, 2, 144], F32), sBr=Res("sB%d" % bs),
                        pz=sbuf(ph, "pz%d" % bs, [128, 8, UW], BF16), pzr=Res("pz%d" % bs),
                        nbank=(0, 7)[bs],
                    )
                    sets.append(d_)
                bk = [0]

                def nextbank():
                    bk[0] = (bk[0] % 6) + 1
                    return bk[0]

                s0 = sets[0]
                h = s0["h"]
                hr = s0["hr"]
                norm_mod(s0["ns"], lambda c: xh[:, c, :], [xhr], 8, 256, D, A1(0), B1(0), lambda c: h[:, c, :256], [hr], 0)
                uhf = uh[:].rearrange("p c a b -> p c (a b)")
                for oc in range(8):
                    b = nextbank()
                    for c in range(8):
                        S.op("pe", lambda e, c=c, oc=oc, b=b: e.matmul(pb[b][:, :256], w_in[:, c, oc * 128:(oc + 1) * 128], h[:, c, :256], start=(c == 0), stop=(c == 7)),
                             reads=[wres, hr], writes=[pbr[b]], inc=(c == 7))
                    S.op("dve", lambda e, oc=oc, b=b: e.tensor_tensor(uhf[:, oc, :], pb[b][:, :256], hmask[:], ALU.mult),
                         reads=[pbr[b], cres], writes=[uhr])

                def unit(u):
                    st_ = sets[u % 2]
                    h, hr, ug, ugr, pz, pzr, ns = st_["h"], st_["hr"], st_["ug"], st_["ugr"], st_["pz"], st_["pzr"], st_["ns"]
                    zt, ztr = h, hr
                    g = u // 2
                    us = slice(u * UW, (u + 1) * UW)
                    norm_mod(ns, lambda c: x[:, c, us], [xr[g]], 8, UW, D, A1(0), B1(0), lambda c: h[:, c, :], [hr], st_["nbank"], src_all=x[:, :, us])
                    yield
                    for oc in range(8):
                        b = nextbank()
                        for c in range(8):
                            S.op("pe", lambda e, c=c, oc=oc, b=b: e.matmul(pb[b][:, :UW], w_in[:, c, oc * 128:(oc + 1) * 128], h[:, c, :], start=(c == 0), stop=(c == 7)),
                                 reads=[wres, hr], writes=[pbr[b]], inc=(c == 7))
                        S.op("act", lambda e, oc=oc, b=b: e.copy(ug[:, oc, :, 16:144], pb[b][:, :UW].rearrange("p (t q) -> p t q", q=128)),
                             reads=[pbr[b]], writes=[ugr[oc]])
                    S.op("dve", lambda e: e.tensor_copy(ug[:, :, :, 0:16], uh[:, :, u * 2:(u + 1) * 2, :]),
                         reads=[uhr], writes=ugr)
                    yield
                    for wgp in range(4):
                        cs = slice(2 * wgp, 2 * wgp + 2)
                        w = 2 << wgp
                        lo = 0
                        step = 1
                        bufs = [(st_["sA"], st_["sAr"]), (st_["sB"], st_["sBr"])]
                        bi = 0
                        ugp = [ugr[2 * wgp], ugr[2 * wgp + 1]]
                        cur, cur_r = ug[:, cs], ugp
                        while step < w:
                            dst, dst_r = bufs[bi]
                            bi ^= 1
                            S.op("dve", lambda e, cur=cur, dst=dst, lo=lo, step=step: e.tensor_tensor(
                                dst[:, :, :, lo + step:144], cur[:, :, :, lo + step:144], cur[:, :, :, lo:144 - step], ALU.add),
                                reads=cur_r, writes=[dst_r])
                            cur, cur_r = dst[:], [dst_r]
                            lo += step
                            step *= 2
                        S.op("dve", lambda e, cur=cur, cs=cs, w=w: e.scalar_tensor_tensor(
                            pz[:, cs, :].rearrange("p c (t q) -> p c t q", q=128), cur[:, :, :, 16:144], 1.0 / w, ug[:, cs, :, 16:144], ALU.mult, ALU.subtract),
                            reads=cur_r + ugp, writes=[pzr])
                        if u == 0:
                            S.op("dve", lambda e, cur=cur, cs=cs: e.tensor_tensor(t16[:], cur[:, :, 0, 16:32], invc[:, cs, :], ALU.mult),
                                 reads=cur_r + [cres], writes=[t16r])
                            S.op("dve", lambda e, cs=cs: e.tensor_tensor(pz[:, cs, 0:16], t16[:], ug[:, cs, 0, 16:32], ALU.subtract),
                                 reads=[t16r, pzr] + ugp, writes=[pzr])
                    yield
                    for wgp in range(4):
                        for oc2 in range(2):
                            b = nextbank()
                            for k2 in range(2):
                                S.op("pe", lambda e, wgp=wgp, oc2=oc2, k2=k2, b=b: e.matmul(
                                    pb[b][:, :UW], w_grp[:, wgp, k2, oc2 * 128:(oc2 + 1) * 128], pz[:, 2 * wgp + k2, :], start=(k2 == 0), stop=(k2 == 1)),
                                    reads=[wres, pzr], writes=[pbr[b]], inc=(k2 == 1))
                            oc = 2 * wgp + oc2
                            S.op("act", lambda e, oc=oc, b=b: e.activation(zt[:, oc, :], pb[b][:, :UW], AF.Identity, scale=col(vecs, VEC_PSC + oc)),
                                 reads=[pbr[b], cres], writes=[ztr])
                    yield
                    for oc in range(8):
                        b = nextbank()
                        for c in range(8):
                            S.op("pe", lambda e, c=c, oc=oc, b=b: e.matmul(pb[b][:, :UW], w_out[:, c, oc * 128:(oc + 1) * 128], zt[:, c, :], start=(c == 0), stop=(c == 7)),
                                 reads=[wres, ztr], writes=[pbr[b]], inc=(c == 7))
                        S.op("dve", lambda e, oc=oc, b=b: e.scalar_tensor_tensor(
                            x[:, oc, us], pb[b][:, :UW], G1(0)(oc), x[:, oc, us], ALU.mult, ALU.add),
                            reads=[pbr[b], mres, xr[g]], writes=[xr[g]])

                run_pipelined([unit(u) for u in range(8)], 2)
                if "x_pool" in dbg:
                    dump("x_pool", x[:], [128, 8, T], F32, xr)
                S.flush()

            with contextlib.ExitStack() as ph:
                h2 = sbuf(ph, "h2", [128, 8, T], BF16)
                h2r = [Res("h2_%d" % g) for g in range(NG)]
                with contextlib.ExitStack() as ph2:
                    ns = NormScratch(ph2, "f")
                    for g in range(NG):
                        norm_mod(ns, lambda c, g=g: x[:, c, g * GW:(g + 1) * GW], [xr[g]], 8, GW, D, A2(0), B2(0),
                                 lambda c, g=g: h2[:, c, g * GW:(g + 1) * GW], [h2r[g]], 7, src_all=x[:, :, g * GW:(g + 1) * GW])
                    S.flush()
                wg_src = lambda e, fb: I["ffn_w_gate"][e][:, fb * 512:(fb + 1) * 512].rearrange("(c p) f -> p c f", p=128)
                wu_src = lambda e, fb: I["ffn_w_up"][e][:, fb * 512:(fb + 1) * 512].rearrange("(c p) f -> p c f", p=128)
                wd_src = lambda e, fb: I["ffn_w_down"][e][fb * 512:(fb + 1) * 512, :].rearrange("(fi p) d -> p fi d", p=128)
                ffn_phase(ph, h2, h2r, wg_src, wu_src, wd_src, 1, G2(0), None)
                if "x_l0" in dbg:
                    dump("x_l0", x[:], [128, 8, T], F32, xr)
                S.flush()

            cs_t, sn_t, ropr = make_tables(tctx, sfx)
            with contextlib.ExitStack() as ph:
                UW = 256
                w_dkv = sbuf(ph, "w_dkv", [128, 8, 256], BF16)
                w_kr2 = sbuf(ph, "w_kr2", [128, 8, 128], BF16)
                wres = Res("kvw")
                S.dma("pool", w_dkv[:], I["w_dkv"].rearrange("(c p) f -> p c f", p=128), writes=[wres])
                S.dma("pool", w_kr2[:], I["w_kr2"].rearrange("(c p) f -> p c f", p=128), writes=[wres])
                Bkv = lambda c: col(modc, 96 + c)
                Akv = lambda c: col(AB, 32 + c)
                lsets = []
                for bs in range(2):
                    lsets.append(dict(
                        ns=NormScratch(ph, "k%d" % bs, n=UW), ns2=NormScratch(ph, "k2%d" % bs, nch=2, n=UW),
                        hk=sbuf(ph, "hk%d" % bs, [128, 8, UW], BF16), hkr=Res("hk%d" % bs),
                        t1=sbuf(ph, "t1%d" % bs, [64, UW], F32), t2=sbuf(ph, "t2%d" % bs, [64, UW], F32),
                        t1r=Res("t1%d" % bs), t2r=Res("t2%d" % bs),
                        latg=sbuf(ph, "latg%d" % bs, [128, 2, UW], BF16), latgr=sbuf(ph, "latgr%d" % bs, [64, UW], BF16),
                        lgr=Res("latg%d" % bs), base=4 * bs))

                def lat_unit(u):
                    st_ = lsets[u % 2]
                    ns, ns2, hk, hkr, t1, t2, t1r, t2r = st_["ns"], st_["ns2"], st_["hk"], st_["hkr"], st_["t1"], st_["t2"], st_["t1r"], st_["t2r"]
                    latg, latgr, lgr, base = st_["latg"], st_["latgr"], st_["lgr"], st_["base"]
                    g = u // 2
                    us = slice(u * UW, (u + 1) * UW)
                    norm_mod(ns, lambda c: x[:, c, us], [xr[g]], 8, UW, D, Akv, Bkv, lambda c: hk[:, c, :], [hkr], base, src_all=x[:, :, us])
                    yield
                    for oc in range(2):
                        for c in range(8):
                            S.op("pe", lambda e, c=c, oc=oc: e.matmul(pb[base + 1][:, oc * UW:(oc + 1) * UW], w_dkv[:, c, oc * 128:(oc + 1) * 128], hk[:, c, :], start=(c == 0), stop=(c == 7)),
                                 reads=[wres, hkr], writes=[pbr[base + 1]], inc=(c == 7))
                    for sw in range(2):
                        for c in range(8):
                            S.op("pe", lambda e, c=c, sw=sw: e.matmul(pb[base + 2][0:64, sw * UW:(sw + 1) * UW], w_kr2[:, c, sw * 64:(sw + 1) * 64], hk[:, c, :], start=(c == 0), stop=(c == 7)),
                                 reads=[wres, hkr], writes=[pbr[base + 2]], inc=(c == 7))
                    yield
                    norm_mod(ns2, lambda c: pb[base + 1][:, c * UW:(c + 1) * UW], [pbr[base + 1]], 2, UW, 256.0, lambda c: col(vecs, VEC_CKVG + c), None,
                             lambda c: latg[:, c, :], [lgr], base + 3)
                    S.op("dve", lambda e: e.tensor_tensor(t1[:], pb[base + 2][0:64, 0:UW], cs_t[:, us], ALU.mult), reads=[pbr[base + 2], ropr], writes=[t1r])
                    S.op("dve", lambda e: e.tensor_tensor(t2[:], pb[base + 2][0:64, UW:2 * UW], sn_t[:, us], ALU.mult), reads=[pbr[base + 2], ropr], writes=[t2r])
                    S.op("dve", lambda e: e.tensor_tensor(latgr[:], t1[:], t2[:], ALU.add), reads=[t1r, t2r], writes=[lgr])
                    S.op("dve", lambda e: e.tensor_copy(Lc5[:, :, 2 * u:2 * u + 2, slot, :], latg[:].rearrange("p c (t q) -> p c t q", q=128)),
                         reads=[lgr], writes=[Lres])
                    S.op("dve", lambda e: e.tensor_copy(Lr4[:, 2 * u:2 * u + 2, slot, :], latgr[:].rearrange("p (t q) -> p t q", q=128)),
                         reads=[lgr], writes=[Lres])

                run_pipelined([lat_unit(u) for u in range(8)], 2)
                S.flush()
            return cs_t, sn_t, ropr

        with contextlib.ExitStack() as tmpctx:
            layer0_pass("_p", 1, tmpctx)
        tctx = st.enter_context(contextlib.ExitStack())
        cs_t, sn_t, ropr = layer0_pass("", 0, tctx)

        if doB:
            with contextlib.ExitStack() as ph:
                cq = sbuf(ph, "cq", [128, 3, T], BF16)
                cqr = Res("cq")
                with contextlib.ExitStack() as ph2:
                    UW = 256
                    w_dq = sbuf(ph2, "w_dq", [128, 8, 384], BF16)
                    wres = Res("wdq")
                    S.dma("pool", w_dq[:], I["w_dq"].rearrange("(c p) f -> p c f", p=128), writes=[wres])
                    qsets = []
                    for bs in range(2):
                        qsets.append(dict(
                            ns=NormScratch(ph2, "a%d" % bs, n=UW), ns3=NormScratch(ph2, "a3%d" % bs, nch=3, n=UW),
                            hq=sbuf(ph2, "hq%d" % bs, [128, 8, UW], BF16), hqr=Res("hq%d" % bs), base=4 * bs))

                    def cq_unit(u):
                        st_ = qsets[u % 2]
                        ns, ns3, hq, hqr, base = st_["ns"], st_["ns3"], st_["hq"], st_["hqr"], st_["base"]
                        g = u // 2
                        us = slice(u * UW, (u + 1) * UW)
                        norm_mod(ns, lambda c: x[:, c, us], [xr[g]], 8, UW, D, A1(1), B1(1), lambda c: hq[:, c, :], [hqr], base, src_all=x[:, :, us])
                        yield
                        for oc in range(3):
                            for c in range(8):
                                S.op("pe", lambda e, c=c, oc=oc: e.matmul(pb[base + 1 + oc // 2][:, (oc % 2) * UW:(oc % 2 + 1) * UW], w_dq[:, c, oc * 128:(oc + 1) * 128], hq[:, c, :],
                                                                         start=(c == 0), stop=(c == 7)),
                                     reads=[wres, hqr], writes=[pbr[base + 1 + oc // 2]], inc=(c == 7))
                        yield
                        norm_mod(ns3, lambda c: pb[base + 1 + c // 2][:, (c % 2) * UW:(c % 2 + 1) * UW], [pbr[base + 1], pbr[base + 2]], 3, UW, 384.0,
                                 lambda c: col(vecs, VEC_QG + c), None, lambda c: cq[:, c, us], [cqr], base + 3)

                    run_pipelined([cq_unit(u) for u in range(8)], 2)
                    S.flush()
                w_uk = sbuf(ph, "w_uk", [128, 2, D], BF16)
                w_uv = sbuf(ph, "w_uv", [128, 2, D], BF16)
                w_uqn = sbuf(ph, "w_uqn", [128, 3, D], BF16)
                w_uqr = sbuf(ph, "w_uqr", [128, 3, D], BF16)
                w_o = sbuf(ph, "w_o", [128, 8, D], BF16)
                amask = sbuf(ph, "amask", [128, 4, 128], BF16)
                wres = Res("attw")
                S.dma("pool", w_uk[:], I["w_uk"].rearrange("(c p) f -> p c f", p=128), writes=[wres])
                S.dma("pool", w_uv[:], I["w_uv"].rearrange("(c p) f -> p c f", p=128), writes=[wres])
                S.dma("pool", w_uqn[:], I["w_uq_n"].rearrange("(c p) f -> p c f", p=128), writes=[wres])
                S.dma("pool", w_uqr[:], I["w_uq_r"].rearrange("(c p) f -> p c f", p=128), writes=[wres])
                S.dma("pool", w_o[:], I["w_o"].rearrange("(c p) f -> p c f", p=128), writes=[wres])
                S.dma("pool", amask[:], I["amask"], writes=[wres])
                KT = sbuf(ph, "KT", [128, 2 * T], BF16)
                KTr = Res("KT")
                Vh = sbuf(ph, "Vh", [128, 32, 128], BF16)
                Vhr = Res("Vh")
                QT = sbuf(ph, "QT", [128, T], BF16)
                QTr = Res("QT")
                QR = sbuf(ph, "QR", [64, T], BF16)
                QRr = Res("QR")
                t1 = sbuf(ph, "qt1", [64, GW], F32)
                t2 = sbuf(ph, "qt2", [64, GW], F32)
                t1r, t2r = Res("qt1"), Res("qt2")
                oT2 = sbuf(ph, "oT", [128, 2, T], BF16)
                oTr2 = [[Res("oT%d_%d" % (i, g)) for g in range(NG)] for i in range(2)]
                pT = [sbuf(ph, "pT%d" % i, [128, GW], BF16) for i in range(4)]
                pTr = [Res("pT%d" % i) for i in range(4)]
                rcp = sbuf(ph, "rcp", [128, GW], F32)
                rcpr = Res("rcp")
                pcnt = 0
                for hd in range(8):
                    hs = slice(hd * 128, (hd + 1) * 128)
                    oT = oT2[:, hd % 2, :]
                    oTr = oTr2[hd % 2]
                    for kg in range(8):
                        b = kg % 2
                        ks = slice(kg * GW, (kg + 1) * GW)
                        for c in range(2):
                            S.op("pe", lambda e, c=c, ks=ks, b=b, hs=hs: e.matmul(pb[b][:], w_uk[:, c, hs], Lc[:, c, ks], start=(c == 0), stop=(c == 1)),
                                 reads=[wres, Lres], writes=[pbr[b]], inc=(c == 1))
                        S.op("act", lambda e, ks=ks, b=b: e.copy(KT[:, ks], pb[b][:]), reads=[pbr[b]], writes=[KTr])
                    for kq in range(8):
                        b = 2 + kq % 2
                        for ti in range(4):
                            kt = kq * 4 + ti
                            for c in range(2):
                                S.op("pe", lambda e, c=c, kt=kt, ti=ti, b=b, hs=hs: e.matmul(
                                    pb[b][:, ti * 128:(ti + 1) * 128], Lc[:, c, kt * 128:(kt + 1) * 128], w_uv[:, c, hs], start=(c == 0), stop=(c == 1)),
                                    reads=[wres, Lres], writes=[pbr[b]], inc=(c == 1 and ti == 3))
                        S.op("dve", lambda e, kq=kq, b=b: e.tensor_copy(Vh[:, kq * 4:(kq + 1) * 4, :], pb[b][:].rearrange("p (t q) -> p t q", q=128)),
                             reads=[pbr[b]], writes=[Vhr])
                    for g in range(NG):
                        gs = slice(g * GW, (g + 1) * GW)
                        b = 4
                        for c in range(3):
                            S.op("pe", lambda e, c=c, gs=gs, hs=hs: e.matmul(pb[4][:], w_uqn[:, c, hs], cq[:, c, gs], start=(c == 0), stop=(c == 2)),
                                 reads=[wres, cqr], writes=[pbr[4]], inc=(c == 2))
                        S.op("act", lambda e, gs=gs: e.copy(QT[:, gs], pb[4][:]), reads=[pbr[4]], writes=[QTr])
                        for sw in range(2):
                            for c in range(3):
                                S.op("pe", lambda e, c=c, gs=gs, sw=sw, hd=hd: e.matmul(
                                    pb[5 + sw][0:64, :], w_uqr[:, c, hd * 128 + sw * 64:hd * 128 + (sw + 1) * 64], cq[:, c, gs], start=(c == 0), stop=(c == 2)),
                                    reads=[wres, cqr], writes=[pbr[5 + sw]], inc=(c == 2))
                        S.op("dve", lambda e, gs=gs: e.tensor_tensor(t1[:], pb[5][0:64, :], cs_t[:, gs], ALU.mult), reads=[pbr[5], ropr], writes=[t1r])
                        S.op("dve", lambda e, gs=gs: e.tensor_tensor(t2[:], pb[6][0:64, :], sn_t[:, gs], ALU.mult), reads=[pbr[6], ropr], writes=[t2r])
                        S.op("dve", lambda e, gs=gs: e.tensor_tensor(QR[:, gs], t1[:], t2[:], ALU.add), reads=[t1r, t2r], writes=[QRr])
                    for m in range(NG):
                        j0 = 4 * m
                        nkt = 8 * m + 8
                        bo, br_ = (2, 3) if m % 2 == 0 else (4, 5)
                        info = {}

                        def emit_S(kt):
                            nonlocal pcnt
                            jmin = max(j0, kt // 2)
                            c0 = (jmin - j0) * 128
                            n = GW - c0
                            qs = slice(m * GW + c0, (m + 1) * GW)
                            bs = kt % 2
                            S.op("pe", lambda e, kt=kt, qs=qs, bs=bs, n=n: e.matmul(pb[bs][:, :n], KT[:, kt * 128:(kt + 1) * 128], QT[:, qs], start=True, stop=False),
                                 reads=[KTr, QTr], writes=[pbr[bs]], inc=False)
                            S.op("pe", lambda e, kt=kt, qs=qs, bs=bs, n=n: e.matmul(pb[bs][:, :n], Lr[:, kt * 128:(kt + 1) * 128], QR[:, qs], start=False, stop=True),
                                 reads=[Lres, QRr], writes=[pbr[bs]])
                            pi_ = pcnt % 4
                            pcnt += 1
                            S.op("act", lambda e, pi_=pi_, bs=bs, n=n: e.activation(pT[pi_][:, :n], pb[bs][:, :n], AF.Exp, scale=ATTN_SCALE),
                                 reads=[pbr[bs]], writes=[pTr[pi_]])
                            if kt // 2 >= j0:
                                mi = 2 * ((kt // 2) % 2) + (kt % 2)
                                S.op("dve", lambda e, pi_=pi_, mi=mi: e.tensor_tensor(pT[pi_][:, 0:128], pT[pi_][:, 0:128], amask[:, mi, :], ALU.mult),
                                     reads=[pTr[pi_], wres], writes=[pTr[pi_]])
                            info[kt] = (pi_, c0, n)

                        def emit_PV(kt):
                            pi_, c0, n = info[kt]
                            S.op("pe", lambda e, kt=kt, pi_=pi_, c0=c0, n=n, bo=bo, nkt=nkt: e.matmul(pb[bo][:, c0:GW], Vh[:, kt, :], pT[pi_][:, :n], start=(kt == 0), stop=(kt == nkt - 1)),
                                 reads=[Vhr, pTr[pi_]], writes=[pbr[bo]], inc=False)
                            S.op("pe", lambda e, kt=kt, pi_=pi_, c0=c0, n=n, br_=br_, nkt=nkt: e.matmul(pb[br_][:, c0:GW], ones[:], pT[pi_][:, :n], start=(kt == 0), stop=(kt == nkt - 1)),
                                 reads=[cres, pTr[pi_]], writes=[pbr[br_]])

                        emit_S(0)
                        for kt in range(nkt):
                            if kt + 1 < nkt:
                                emit_S(kt + 1)
                            emit_PV(kt)
                        S.op("act", lambda e, br_=br_: e.activation(rcp[:], pb[br_][:], AF.Ln), reads=[pbr[br_]], writes=[rcpr])
                        S.op("act", lambda e: e.activation(rcp[:], rcp[:], AF.Exp, scale=-1.0), reads=[rcpr], writes=[rcpr])
                        S.op("dve", lambda e, m=m, bo=bo, oT=oT: e.tensor_tensor(oT[:, m * GW:(m + 1) * GW], pb[bo][:], rcp[:], ALU.mult),
                             reads=[pbr[bo], rcpr], writes=[oTr[m]])
                    if "oT0" in dbg and hd == 0:
                        dump("oT0", oT[:], [128, T], BF16, oTr)
                        dump("QT0", QT[:], [128, T], BF16, [QTr])
                        dump("QR0", QR[:], [64, T], BF16, [QRr])
                        dump("KT0", KT[:], [128, 2 * T], BF16, [KTr])
                    if hd % 2 == 0:
                        continue
                    for g in range(NG):
                        gs = slice(g * GW, (g + 1) * GW)
                        for dc in range(8):
                            b = 6 + dc % 2
                            for hh in range(2):
                                S.op("pe", lambda e, dc=dc, gs=gs, b=b, hd=hd, hh=hh: e.matmul(
                                    pb[b][:], w_o[:, hd - 1 + hh, dc * 128:(dc + 1) * 128], oT2[:, hh, gs], start=(hh == 0), stop=(hh == 1)),
                                    reads=[wres, oTr2[hh][g]], writes=[pbr[b]], inc=(hh == 1))
                            S.op("dve", lambda e, dc=dc, gs=gs, b=b: e.scalar_tensor_tensor(x[:, dc, gs], pb[b][:], G1(1)(dc), x[:, dc, gs], ALU.mult, ALU.add),
                                 reads=[pbr[b], mres, xr[g]], writes=[xr[g]])
                if "x_att" in dbg:
                    dump("x_att", x[:], [128, 8, T], F32, xr)
                S.flush()
            tctx.close()
            Lctx.close()

            with contextlib.ExitStack() as ph:
                h2 = sbuf(ph, "h2m", [128, 8, T], BF16)
                h2r = [Res("h2m_%d" % g) for g in range(NG)]
                gT = sbuf(ph, "gT", [8, T], F32)
                gTr = Res("gT")
                esel = sbuf(ph, "esel", [8, 8, 128], F32)
                S.dma("sp", esel[:], I["esel"], writes=[cres])
                with contextlib.ExitStack() as ph2:
                    ns = NormScratch(ph2, "m")
                    h32 = sbuf(ph2, "h32", [128, 8, GW], F32)
                    h32r = Res("h32")
                    rw = sbuf(ph2, "rw", [128, 8, 8], F32)
                    rwr = Res("rw")
                    S.dma("sp", rw[:], I["router_w"].rearrange("(c p) e -> p c e", p=128), writes=[rwr])
                    L = sbuf(ph2, "L", [128, 16, 8], F32)
                    L2 = sbuf(ph2, "L2", [128, 16, 8], F32)
                    eq1 = sbuf(ph2, "eq1", [128, 16, 8], F32)
                    eq2 = sbuf(ph2, "eq2", [128, 16, 8], F32)
                    m1 = sbuf(ph2, "m1", [128, 16], F32)
                    m2 = sbuf(ph2, "m2", [128, 16], F32)
                    w1 = sbuf(ph2, "w1", [128, 16], F32)
                    w2 = sbuf(ph2, "w2", [128, 16], F32)
                    gates = sbuf(ph2, "gates", [128, 16, 8], F32)
                    rr = Res("router")
                    lview = pb[7][:, 0:128].rearrange("p (a b) -> p a b", b=8)
                    for g in range(NG):
                        gs = slice(g * GW, (g + 1) * GW)
                        norm_mod(ns, lambda c, gs=gs: x[:, c, gs], [xr[g]], 8, GW, D, A2(1), B2(1), lambda c: h32[:, c, :], [h32r], 6, src_all=x[:, :, gs])
                        S.op("act", lambda e, gs=gs: e.copy(h2[:, :, gs], h32[:]), reads=[h32r], writes=[h2r[g]])
                        for ti in range(4):
                            tt = g * 4 + ti
                            for c in range(8):
                                S.op("pe", lambda e, c=c, ti=ti, tt=tt: e.matmul(lview[:, tt, :], h32[:, c, ti * 128:(ti + 1) * 128], rw[:, c, :], start=(c == 0), stop=(c == 7)),
                                     reads=[h32r, rwr], writes=[pbr[7]], inc=(c == 7))
                    S.op("dve", lambda e: e.tensor_copy(L[:], lview), reads=[pbr[7]], writes=[rr])
                    bc = lambda t: t[:].unsqueeze(2).to_broadcast([128, 16, 8])
                    S.op("dve", lambda e: e.tensor_reduce(m1[:], L[:], AX.X, ALU.max), reads=[rr], writes=[rr])
                    S.op("dve", lambda e: e.tensor_tensor(eq1[:], L[:], bc(m1), ALU.is_equal), reads=[rr], writes=[rr])
                    S.op("dve", lambda e: e.scalar_tensor_tensor(L2[:], eq1[:], -1e30, L[:], ALU.mult, ALU.add), reads=[rr], writes=[rr])
                    S.op("dve", lambda e: e.tensor_reduce(m2[:], L2[:], AX.X, ALU.max), reads=[rr], writes=[rr])
                    S.op("dve", lambda e: e.tensor_tensor(eq2[:], L2[:], bc(m2), ALU.is_equal), reads=[rr], writes=[rr])
                    S.op("dve", lambda e: e.tensor_tensor(w2[:], m2[:], m1[:], ALU.subtract), reads=[rr], writes=[rr])
                    S.op("act", lambda e: e.activation(w2[:], w2[:], AF.Exp), reads=[rr], writes=[rr])
                    S.op("dve", lambda e: e.tensor_scalar(w1[:], w2[:], 1.0, None, ALU.add), reads=[rr], writes=[rr])
                    S.op("dve", lambda e: e.reciprocal(w1[:], w1[:]), reads=[rr], writes=[rr])
                    S.op("dve", lambda e: e.tensor_tensor(w2[:], w2[:], w1[:], ALU.mult), reads=[rr], writes=[rr])
                    S.op("dve", lambda e: e.tensor_tensor(gates[:], eq1[:], bc(w1), ALU.mult), reads=[rr], writes=[rr])
                    S.op("dve", lambda e: e.tensor_tensor(eq2[:], eq2[:], bc(w2), ALU.mult), reads=[rr], writes=[rr])
                    S.op("dve", lambda e: e.tensor_tensor(gates[:], gates[:], eq2[:], ALU.add), reads=[rr], writes=[rr])
                    for tt in range(16):
                        b = 4 + (tt // 4) % 2
                        S.op("pe", lambda e, tt=tt, b=b: e.transpose(pb[b][0:8, (tt % 4) * 128:(tt % 4 + 1) * 128], gates[:, tt, :], ident[:]),
                             reads=[rr, cres], writes=[pbr[b]], inc=(tt % 4 == 3))
                        if tt % 4 == 3:
                            g = tt // 4
                            S.op("dve", lambda e, g=g, b=b: e.tensor_copy(gT[:, g * GW:(g + 1) * GW], pb[b][0:8, :]), reads=[pbr[b]], writes=[gTr])
                    if "gT" in dbg:
                        dump("gT", gT[:], [8, T], F32, [gTr])
                    S.flush()
                gwb = [sbuf(ph, "gwb%d" % i, [128, T], BF16) for i in range(2)]
                gwbr = [Res("gwb%d" % i) for i in range(2)]

                class GateFn:
                    cur = None

                    def __call__(self, e):
                        i = e % 2
                        for g in range(NG):
                            b = 7
                            S.op("pe", lambda e_, g=g, b=b, e=e: e_.matmul(pb[b][:], esel[:, e, :], gT[:, g * GW:(g + 1) * GW], start=True, stop=True),
                                 reads=[cres, gTr], writes=[pbr[b]])
                            S.op("act", lambda e_, g=g, b=b, i=i: e_.copy(gwb[i][:, g * GW:(g + 1) * GW], pb[b][:]),
                                 reads=[pbr[b]], writes=[gwbr[i]])
                        self.cur = (gwb[i], gwbr[i])
                        return self.cur

                wg_src = lambda e, fb: I["moe_w_gate"][e][:, fb * 512:(fb + 1) * 512].rearrange("(c p) f -> p c f", p=128)
                wu_src = lambda e, fb: I["moe_w_up"][e][:, fb * 512:(fb + 1) * 512].rearrange("(c p) f -> p c f", p=128)
                wd_src = lambda e, fb: I["moe_w_down"][e][fb * 512:(fb + 1) * 512, :].rearrange("(fi p) d -> p fi d", p=128)
                ffn_phase(ph, h2, h2r, wg_src, wu_src, wd_src, NE, G2(1), GateFn())
                if "x_moe" in dbg:
                    dump("x_moe", x[:], [128, 8, T], F32, xr)
                S.flush()

            with contextlib.ExitStack() as ph:
                ns = NormScratch(ph, "o")
                ob = [sbuf(ph, "ob%d" % i, [128, 8, GW], F32) for i in range(2)]
                obr = [Res("ob%d" % i) for i in range(2)]
                odst = O["outT"].rearrange("(c p) t -> p c t", p=128)
                for g in range(NG):
                    gs = slice(g * GW, (g + 1) * GW)
                    i = g % 2
                    norm_mod(ns, lambda c, gs=gs: x[:, c, gs], [xr[g]], 8, GW, D, lambda c: col(vecs, VEC_FING + c), None,
                             lambda c, i=i: ob[i][:, c, :], [obr[i]], g % 2)
                    S.dma("sp", odst[:, :, gs], ob[i][:], reads=[obr[i]])
                S.flush()
    return nc, dbg_out


def tile_map(half):
    if half == 0:
        return [2 * j + (j % 2) for j in range(NT)]
    return [2 * j + 1 - (j % 2) for j in range(NT)]


def _common_inputs(inp):
    f32 = np.float32
    vecs = np.zeros((128, NVEC), f32)

    def colv(v):
        return np.asarray(v, f32).reshape(-1, 128).T

    vecs[:, 0:8] = colv(inp["mix_norm_g"][0])
    vecs[:, 8:16] = colv(inp["mix_norm_g"][1])
    vecs[:, 16:24] = colv(inp["ffn_norm_g"][0])
    vecs[:, 24:32] = colv(inp["ffn_norm_g"][1])
    vecs[:, 32:40] = colv(inp["kv_norm_g"])
    vecs[:, 40:48] = colv(inp["pool_scale"][0])
    vecs[:, 48:56] = colv(inp["final_norm_g"])
    vecs[:, 56:58] = colv(inp["ckv_norm_g"])
    vecs[:, 58:61] = colv(inp["q_norm_g"][0])
    inv_freq = (10000.0 ** (-(np.arange(0, 64, 2, dtype=f32)) / 64)).astype(f32)
    ropec = np.zeros((64, 2), f32)
    ropec[:, 0] = np.concatenate([inv_freq, inv_freq])
    ropec[:32, 1] = -1.0
    ropec[32:, 1] = 1.0
    perm = np.concatenate([np.arange(32, 64), np.arange(0, 32)])
    w_kr = np.asarray(inp["w_kr"], f32)
    w_kr2 = np.ascontiguousarray(np.concatenate([w_kr, w_kr[:, perm]], axis=1))
    w_uq = np.asarray(inp["w_uq"][0], f32).reshape(384, 8, 192)
    w_uq_n = np.ascontiguousarray(w_uq[:, :, :128].reshape(384, D))
    rp = w_uq[:, :, 128:]
    w_uq_r = np.ascontiguousarray(np.concatenate([rp, rp[:, :, perm]], axis=2).reshape(384, D))
    esel = np.zeros((8, 8, 128), f32)
    for e in range(8):
        esel[e, e, :] = 1.0
    return dict(vecs=vecs, ropec=ropec, w_kr2=w_kr2, w_uq_n=w_uq_n, w_uq_r=w_uq_r, esel=esel,
                ident=np.eye(128, dtype=f32))


def _token_inputs(inp, b, half, sfx):
    f32 = np.float32
    tm = tile_map(half)
    d = {}
    xb = np.asarray(inp["x"][b], f32)
    xt = xb.reshape(32, 128, D)[tm].reshape(T, D)
    d["xT" + sfx] = np.ascontiguousarray(xt.T)
    halo = np.zeros((NT, 16, D), f32)
    hmask = np.ones((128, NT, 16), f32)
    for j, gt in enumerate(tm):
        if gt == 0:
            hmask[:, j, :] = 0.0
        else:
            halo[j] = xb[gt * 128 - 16:gt * 128]
    d["xhT" + sfx] = np.ascontiguousarray(halo.reshape(256, D).T)
    d["hmask" + sfx] = hmask.reshape(128, 256)
    invc = np.zeros((128, 8, 16), f32)
    for c in range(8):
        w = 2 << (c // 2)
        if tm[0] == 0:
            invc[:, c, :] = 1.0 / np.minimum(np.arange(1, 17), w).astype(f32)
        else:
            invc[:, c, :] = 1.0 / w
    d["invc" + sfx] = invc
    d["pos" + sfx] = np.asarray(inp["positions"][b]).reshape(32, 128)[tm].reshape(1, T).astype(np.int32)
    return d


def _core_inputs(inp, com, shared, tok, b, half):
    f32 = np.float32
    tm = tile_map(half)
    d = dict(shared)
    d.update(tok[(b, half, "")])
    d.update(tok[(b, 1 - half, "_p")])
    d["cT"] = np.ascontiguousarray(np.asarray(inp["c"][b], f32).reshape(8, 128).T)
    tri = (np.arange(128)[:, None] <= np.arange(128)[None, :]).astype(f32)
    am = np.zeros((128, 4, 128), f32)
    for par in range(2):
        gt_par = tm[par] % 2
        am[:, 2 * par + 0, :] = tri
        am[:, 2 * par + 1, :] = 1.0 if gt_par == 1 else 0.0
    d["amask"] = am
    return d


def _shared_inputs(inp, com):
    f32 = np.float32
    d = {}
    d["vecs"] = com["vecs"]
    d["ident"] = com["ident"]
    d["ropec"] = com["ropec"]
    d["esel"] = com["esel"]
    d["w_kr2"] = com["w_kr2"]
    d["w_uq_n"] = com["w_uq_n"]
    d["w_uq_r"] = com["w_uq_r"]
    d["ada_w"] = np.asarray(inp["ada_w"], f32)
    d["ada_b"] = np.asarray(inp["ada_b"], f32).reshape(1, 12 * D)
    d["pool_w_in"] = np.asarray(inp["pool_w_in"][0], f32)
    d["pool_w_grp"] = np.asarray(inp["pool_w_grp"][0], f32)
    d["pool_w_out"] = np.asarray(inp["pool_w_out"][0], f32)
    d["ffn_w_gate"] = np.asarray(inp["ffn_w_gate"], f32)
    d["ffn_w_up"] = np.asarray(inp["ffn_w_up"], f32)
    d["ffn_w_down"] = np.asarray(inp["ffn_w_down"], f32)
    d["kv_ada_w"] = np.asarray(inp["kv_ada_w"], f32)
    d["kv_ada_b"] = np.asarray(inp["kv_ada_b"], f32).reshape(1, 2 * D)
    d["w_dkv"] = np.asarray(inp["w_dkv"], f32)
    d["w_uk"] = np.asarray(inp["w_uk"], f32)
    d["w_uv"] = np.asarray(inp["w_uv"], f32)
    d["w_dq"] = np.asarray(inp["w_dq"][0], f32)
    d["w_o"] = np.asarray(inp["w_o"][0], f32)
    d["router_w"] = np.asarray(inp["router_w"][0], f32)
    d["moe_w_gate"] = np.asarray(inp["moe_w_gate"][0], f32)
    d["moe_w_up"] = np.asarray(inp["moe_w_up"][0], f32)
    d["moe_w_down"] = np.asarray(inp["moe_w_down"][0], f32)
    return d


_PROGS = {}


def _prog(mode="AB"):
    if mode not in _PROGS:
        _PROGS[mode] = build(mode)[0]
    return _PROGS[mode]


def make_in_maps(inp):
    com = _common_inputs(inp)
    shared = _shared_inputs(inp, com)
    cores = [(b, h) for b in range(4) for h in range(2)]
    tok = {}
    for (b, h) in cores:
        for sfx in ("", "_p"):
            tok[(b, h, sfx)] = _token_inputs(inp, b, h, sfx)
    return cores, [_core_inputs(inp, com, shared, tok, b, h) for (b, h) in cores]


def kernel(**inp):
    cores, maps = make_in_maps(inp)
    res = run_bass_kernel_spmd(_prog(), maps, core_ids=list(range(8))).results
    out = np.zeros((4, 32, 128, D), np.float32)
    for ci, (b, h) in enumerate(cores):
        tm = tile_map(h)
        out[b, tm] = np.asarray(res[ci]["outT"]).T.reshape(NT, 128, D)
    return out.reshape(4, 4096, D)
```

```python
import contextlib
import numpy as np
import ml_dtypes
import concourse.bass as bass
import concourse.mybir as mybir
from concourse.bass_utils import run_bass_kernel_spmd

F32 = mybir.dt.float32
BF16 = mybir.dt.bfloat16
I32 = mybir.dt.int32
ALU = mybir.AluOpType
AF = mybir.ActivationFunctionType
AX = mybir.AxisListType

ENGS = ("pe", "act", "dve", "pool", "sp")

D = 1024
T = 2048
NT = 16
NG = 4
GW = 512
DFF = 3584
NF = 28
FB = 4
NFB = NF // FB
NE = 8
EPS = 1e-6
ATTN_SCALE = 1.0 / float(np.sqrt(192.0))
PI = float(np.pi)


class Res:
    __slots__ = ("w", "r", "name")

    def __init__(self, name=""):
        self.w = None
        self.r = {}
        self.name = name


class Sched:
    def __init__(self, nc, stack, n_dma=32, self_wait=True):
        self.nc = nc
        self.ops = {e: [] for e in ENGS}
        self.cnt = {e: 0 for e in ENGS}
        self.seen = {e: {} for e in ENGS}
        self.sems = {}
        for e in ("pe", "act", "dve", "pool"):
            self.sems[e] = stack.enter_context(nc.semaphore("s_" + e))
        self.n_dma = n_dma
        for i in range(n_dma):
            self.sems[("d", i)] = stack.enter_context(nc.semaphore("s_d%d" % i))
        self.dma_cnt = [0] * n_dma
        self.dma_rr = 0
        self.self_wait = self_wait
        self.out_ticks = []

    def _waits(self, eng, reads, writes, extra=()):
        waits = {}

        def need(t):
            if t is None:
                return
            k, v = t
            if k == eng and (eng == "pe" or not self.self_wait):
                return
            if waits.get(k, 0) < v:
                waits[k] = v

        for r in reads:
            need(r.w)
        for w in writes:
            if w.w is not None and w.w[0] != eng:
                need(w.w)
            for k, v in w.r.items():
                if k != eng:
                    need((k, v))
        for t in extra:
            need(t)
        seen = self.seen[eng]
        wl = []
        for k, v in waits.items():
            if seen.get(k, 0) >= v:
                continue
            seen[k] = v
            wl.append((k, v))
        return wl

    def op(self, eng, fn, reads=(), writes=(), inc=True):
        wl = self._waits(eng, reads, writes)
        if inc:
            self.cnt[eng] += 1
            tick = (eng, self.cnt[eng])
        else:
            tick = (eng, self.cnt[eng] + 1)
        self.ops[eng].append((fn, wl, eng if inc else None))
        for r in reads:
            if r.r.get(eng, 0) < tick[1]:
                r.r[eng] = tick[1]
        for w in writes:
            w.w = tick
            w.r = {}
        return tick

    def dma(self, q, out, in_, reads=(), writes=(), **kw):
        i = self.dma_rr
        self.dma_rr = (i + 1) % self.n_dma
        key = ("d", i)
        prev = (key, 16 * self.dma_cnt[i]) if self.dma_cnt[i] else None
        wl = self._waits(q, reads, writes, extra=(prev,) if prev else ())
        self.dma_cnt[i] += 1
        tick = (key, 16 * self.dma_cnt[i])

        def fn(e, out=out, in_=in_, kw=kw):
            return e.dma_start(out=out, in_=in_, **kw)

        self.ops[q].append((fn, wl, key))
        for r in reads:
            if r.r.get(key, 0) < tick[1]:
                r.r[key] = tick[1]
        for w in writes:
            w.w = tick
            w.r = {}
        return tick

    def raw(self, q, fn, key_inc=None, reads=(), writes=(), extra=()):
        i = self.dma_rr
        self.dma_rr = (i + 1) % self.n_dma
        key = ("d", i)
        prev = (key, 16 * self.dma_cnt[i]) if self.dma_cnt[i] else None
        wl = self._waits(q, reads, writes, extra=tuple(extra) + ((prev,) if prev else ()))
        self.dma_cnt[i] += 1
        tick = (key, 16 * self.dma_cnt[i])
        self.ops[q].append((fn, wl, key))
        for r in reads:
            if r.r.get(key, 0) < tick[1]:
                r.r[key] = tick[1]
        for w in writes:
            w.w = tick
            w.r = {}
        return tick

    def wait_all(self, eng, tickets):
        wl = self._waits(eng, (), (), extra=tickets)
        self.ops[eng].append((None, wl, None))

    def flush(self):
        allt = [(("d", i), 16 * c) for i, c in enumerate(self.dma_cnt) if c]
        self.wait_all("sp", allt)
        fin = [(k, self.cnt[k]) for k in ("pe", "act", "dve", "pool") if self.cnt[k]]
        for eng in ENGS:
            self.wait_all(eng, [t for t in fin if t[0] != eng])
        nc = self.nc
        handles = {"pe": "tensor", "act": "scalar", "dve": "vector", "pool": "gpsimd", "sp": "sync"}
        with nc.Block() as block:
            for ename in ENGS:
                ops = self.ops[ename]
                if not ops:
                    continue

                def body(e, ops=ops):
                    for fn, wl, inc in ops:
                        for k, v in wl:
                            e.wait_ge(self.sems[k], v)
                        if fn is None:
                            continue
                        ins = fn(e)
                        if inc is not None:
                            ins.then_inc(self.sems[inc], 16 if isinstance(inc, tuple) else 1)

                getattr(block, handles[ename])(body)
        self.ops = {e: [] for e in ENGS}


def run_pipelined(gens, depth=2):
    it = iter(gens)
    active = []
    exhausted = False
    while True:
        for g in list(active):
            try:
                next(g)
            except StopIteration:
                active.remove(g)
        if not exhausted and len(active) < depth:
            try:
                g = next(it)
                active.append(g)
                next(g)
            except StopIteration:
                exhausted = True
        if exhausted and not active:
            break


VEC_MIXG = (0, 8)
VEC_FFNG = 16
VEC_KVG = 32
VEC_PSC = 40
VEC_FING = 48
VEC_CKVG = 56
VEC_QG = 58
NVEC = 64


def build(mode="AB", dbg=()):
    nc = bass.Bass("TRN2", target_bir_lowering=False)
    doA = True
    doB = True

    def din(name, shape, dt=F32):
        return nc.dram_tensor(name, list(shape), dt, kind="ExternalInput").ap()

    def dout(name, shape, dt=F32):
        return nc.dram_tensor(name, list(shape), dt, kind="ExternalOutput").ap()

    I = {}
    for sfx in ("", "_p"):
        I["xT" + sfx] = din("xT" + sfx, [D, T])
        I["xhT" + sfx] = din("xhT" + sfx, [D, 256])
        I["hmask" + sfx] = din("hmask" + sfx, [128, 256])
        I["invc" + sfx] = din("invc" + sfx, [128, 8, 16])
        I["pos" + sfx] = din("pos" + sfx, [1, T], I32)
    I["cT"] = din("cT", [128, 8])
    I["vecs"] = din("vecs", [128, NVEC])
    I["ident"] = din("ident", [128, 128])
    I["ada_w"] = din("ada_w", [2, D, 6 * D])
    I["ada_b"] = din("ada_b", [1, 12 * D])
    I["pool_w_in"] = din("pool_w_in", [D, D])
    I["pool_w_grp"] = din("pool_w_grp", [4, 256, 256])
    I["pool_w_out"] = din("pool_w_out", [D, D])
    I["ffn_w_gate"] = din("ffn_w_gate", [1, D, DFF])
    I["ffn_w_up"] = din("ffn_w_up", [1, D, DFF])
    I["ffn_w_down"] = din("ffn_w_down", [1, DFF, D])
    I["kv_ada_w"] = din("kv_ada_w", [D, 2 * D])
    I["kv_ada_b"] = din("kv_ada_b", [1, 2 * D])
    I["w_dkv"] = din("w_dkv", [D, 256])
    I["w_kr2"] = din("w_kr2", [D, 128])
    I["ropec"] = din("ropec", [64, 2])
    I["amask"] = din("amask", [128, 4, 128])
    I["esel"] = din("esel", [8, 8, 128])
    I["w_uk"] = din("w_uk", [256, D])
    I["w_uv"] = din("w_uv", [256, D])
    I["w_dq"] = din("w_dq", [D, 384])
    I["w_uq_n"] = din("w_uq_n", [384, D])
    I["w_uq_r"] = din("w_uq_r", [384, D])
    I["w_o"] = din("w_o", [D, D])
    I["router_w"] = din("router_w", [D, 8])
    I["moe_w_gate"] = din("moe_w_gate", [NE, D, DFF])
    I["moe_w_up"] = din("moe_w_up", [NE, D, DFF])
    I["moe_w_down"] = din("moe_w_down", [NE, DFF, D])
    O = {}
    O["outT"] = dout("outT", [D, T])
    dbg_out = {}

    with contextlib.ExitStack() as st:
        S = Sched(nc, st)

        _uid = [0]

        def sbuf(stack, name, shape, dt):
            _uid[0] += 1
            return stack.enter_context(nc.sbuf_tensor("sb%d_%s" % (_uid[0], name), list(shape), dt))

        pb = [st.enter_context(nc.psum_tensor("pb%d" % i, [128, 512], F32)) for i in range(8)]
        pbr = [Res("pb%d" % i) for i in range(8)]
        x = sbuf(st, "x", [128, 8, T], F32)
        xr = [Res("x%d" % g) for g in range(NG)]
        vecs = sbuf(st, "vecs", [128, NVEC], F32)
        modc = sbuf(st, "modc", [128, 112], F32)
        AB = sbuf(st, "ABv", [128, 64], F32)
        ident = sbuf(st, "ident", [128, 128], F32)
        identb = sbuf(st, "identb", [128, 128], BF16)
        ones = sbuf(st, "ones", [128, 128], BF16)
        onesf = sbuf(st, "onesf", [128, 128], F32)
        epsD = sbuf(st, "epsD", [128, 1], F32)
        cres = Res("consts")
        mres = Res("modc")

        S.dma("sp", vecs[:], I["vecs"], writes=[cres])
        S.dma("sp", ident[:], I["ident"], writes=[cres])
        S.dma("pool", identb[:], I["ident"], writes=[cres])
        S.op("dve", lambda e: e.memset(ones[:], 1.0), writes=[cres])
        S.op("dve", lambda e: e.memset(onesf[:], 1.0), writes=[cres])
        S.op("dve", lambda e: e.memset(epsD[:], EPS), writes=[cres])
        Lctx = st.enter_context(contextlib.ExitStack())
        Lc = sbuf(Lctx, "Lc", [128, 2, 2 * T], BF16)
        Lr = sbuf(Lctx, "Lr", [64, 2 * T], BF16)
        Lres = Res("L")
        Lc5 = Lc[:].rearrange("p c (i s q) -> p c i s q", s=2, q=128)
        Lr4 = Lr[:].rearrange("p (i s q) -> p i s q", s=2, q=128)

        def dump(name, ap, shape, dt, res):
            o = dout("dbg_" + name, shape, dt)
            dbg_out[name] = o
            S.dma("sp", o, ap, reads=res)

        class NormScratch:
            def __init__(self, stack, tag, nch=8, n=GW, tmp=None, r_tmp=None):
                self.sq = sbuf(stack, "sq" + tag, [128, nch, n], BF16)
                self.tmp = tmp if tmp is not None else sbuf(stack, "tmp" + tag, [128, nch, n], F32)
                self.rstd = sbuf(stack, "rstd" + tag, [128, n], F32)
                self.r_sq = Res("sq" + tag)
                self.r_tmp = r_tmp if r_tmp is not None else [Res("tmp%s_%d" % (tag, c)) for c in range(nch)]
                self.r_rstd = Res("rstd" + tag)

        def norm_mod(ns, src, src_res, nch, n, dfeat, A, B, out, out_res, bank, extra_reads=(), src_all=None):
            if src_all is not None:
                S.op("act", lambda e: e.activation(ns.sq[:, :, :n], src_all, AF.Square),
                     reads=list(src_res) + list(extra_reads), writes=[ns.r_sq])
            for c in range(nch if src_all is None else 0):
                S.op("act", lambda e, c=c: e.activation(ns.sq[:, c, :n], src(c), AF.Square),
                     reads=list(src_res) + list(extra_reads), writes=[ns.r_sq])
            for c in range(nch):
                S.op("pe", lambda e, c=c: e.matmul(pb[bank][:, :n], ones[:], ns.sq[:, c, :n], start=(c == 0), stop=(c == nch - 1)),
                     reads=[ns.r_sq, cres], writes=[pbr[bank]], inc=(c == nch - 1))
            S.op("act", lambda e: e.activation(ns.rstd[:, :n], pb[bank][:, :n], AF.Sqrt, bias=epsD[:, 0:1], scale=1.0 / dfeat),
                 reads=[pbr[bank], cres], writes=[ns.r_rstd])
            S.op("dve", lambda e: e.reciprocal(ns.rstd[:, :n], ns.rstd[:, :n]), reads=[ns.r_rstd], writes=[ns.r_rstd])
            for c in range(nch):
                if B is None:
                    S.op("dve", lambda e, c=c: e.scalar_tensor_tensor(out(c), src(c), A(c), ns.rstd[:, :n], ALU.mult, ALU.mult),
                         reads=list(src_res) + [ns.r_rstd, mres], writes=list(out_res))
                else:
                    S.op("dve", lambda e, c=c: e.scalar_tensor_tensor(ns.tmp[:, c, :n], src(c), A(c), ns.rstd[:, :n], ALU.mult, ALU.mult),
                         reads=list(src_res) + [ns.r_rstd, mres], writes=[ns.r_tmp[c]])
                    S.op("act", lambda e, c=c: e.activation(out(c), ns.tmp[:, c, :n], AF.Identity, bias=B(c), scale=1.0),
                         reads=[ns.r_tmp[c], mres], writes=list(out_res))

        def col(t, j):
            return t[:, j:j + 1]

        with contextlib.ExitStack() as ph:
            sc = sbuf(ph, "sc", [128, 8], F32)
            scr = Res("sc")
            S.dma("sp", sc[:], I["cT"], writes=[scr])
            S.op("act", lambda e: e.activation(sc[:], sc[:], AF.Silu), reads=[scr], writes=[scr])
            blocks = [("ada", l, j) for l in range(2) for j in range(12)]
            if doA:
                blocks += [("kv", 0, j) for j in range(4)]
            wa = [sbuf(ph, "wa%d" % i, [128, 8, 512], BF16) for i in range(4)]
            scb = sbuf(ph, "scb", [128, 8], BF16)
            S.op("act", lambda e: e.copy(scb[:], sc[:]), reads=[scr], writes=[scr])
            war = [Res("wa%d" % i) for i in range(4)]
            brow = [sbuf(ph, "brow%d" % i, [1, 512], F32) for i in range(4)]
            mrow = [sbuf(ph, "mrow%d" % i, [1, 512], F32) for i in range(2)]
            mrowr = [Res("mrow%d" % i) for i in range(2)]
            ncol = 112 if doA else 96
            for bi, (kind, l, j) in enumerate(blocks):
                s = bi % 4
                if kind == "ada":
                    src = I["ada_w"][l][:, j * 512:(j + 1) * 512].rearrange("(c p) f -> p c f", p=128)
                    off = l * 6 * D + j * 512
                    bsrc = I["ada_b"][:, off:off + 512]
                else:
                    src = I["kv_ada_w"][:, j * 512:(j + 1) * 512].rearrange("(c p) f -> p c f", p=128)
                    off = 12 * D + j * 512
                    bsrc = I["kv_ada_b"][:, j * 512:(j + 1) * 512]
                S.dma("pool", wa[s][:], src, writes=[war[s]])
                S.dma("sp", brow[s][:], bsrc, writes=[war[s]])
                bank = bi % 2
                for c in range(8):
                    S.op("pe", lambda e, c=c, s=s, bank=bank: e.matmul(pb[bank][0:1, :], scb[:, c:c + 1], wa[s][:, c, :], start=(c == 0), stop=(c == 7)),
                         reads=[scr, war[s]], writes=[pbr[bank]], inc=(c == 7))
                S.op("dve", lambda e, s=s, bank=bank: e.tensor_tensor(mrow[bank][0:1, :], pb[bank][0:1, :], brow[s][0:1, :], ALU.add),
                     reads=[pbr[bank], war[s]], writes=[mrowr[bank]])
                for q in range(4):
                    jj = off // 128 + q
                    S.op("pe", lambda e, jj=jj, q=q, bank=bank: e.matmul(pb[2][:, jj:jj + 1], mrow[bank][0:1, q * 128:(q + 1) * 128], onesf[0:1, 0:1], start=True, stop=True),
                         reads=[mrowr[bank], cres], writes=[pbr[2]], inc=(q == 3))
            S.op("dve", lambda e: e.tensor_copy(modc[:, :ncol], pb[2][:, :ncol]), reads=[pbr[2]], writes=[mres])
            for l in range(2):
                S.op("dve", lambda e, l=l: e.scalar_tensor_tensor(AB[:, l * 16:l * 16 + 8], modc[:, l * 48 + 8:l * 48 + 16], 1.0,
                                                                   vecs[:, VEC_MIXG[l]:VEC_MIXG[l] + 8], ALU.add, ALU.mult),
                     reads=[mres, cres], writes=[mres])
                S.op("dve", lambda e, l=l: e.scalar_tensor_tensor(AB[:, l * 16 + 8:l * 16 + 16], modc[:, l * 48 + 32:l * 48 + 40], 1.0,
                                                                   vecs[:, VEC_FFNG + l * 8:VEC_FFNG + l * 8 + 8], ALU.add, ALU.mult),
                     reads=[mres, cres], writes=[mres])
            if doA:
                S.op("dve", lambda e: e.scalar_tensor_tensor(AB[:, 32:40], modc[:, 104:112], 1.0, vecs[:, VEC_KVG:VEC_KVG + 8], ALU.add, ALU.mult),
                     reads=[mres, cres], writes=[mres])
            if "modc" in dbg:
                dump("modc", modc[:], [128, 112], F32, [mres])
            S.flush()

        def A1(l):
            return lambda c: col(AB, l * 16 + c)

        def B1(l):
            return lambda c: col(modc, l * 48 + c)

        def G1(l):
            return lambda c: col(modc, l * 48 + 16 + c)

        def A2(l):
            return lambda c: col(AB, l * 16 + 8 + c)

        def B2(l):
            return lambda c: col(modc, l * 48 + 24 + c)

        def G2(l):
            return lambda c: col(modc, l * 48 + 40 + c)

        def make_tables(ctx, sfx):
            cs_t = sbuf(ctx, "cs_t", [64, T], F32)
            sn_t = sbuf(ctx, "sn_t", [64, T], F32)
            ropr = Res("rope")
            with contextlib.ExitStack() as ph:
                posi = sbuf(ph, "posi", [64, T], I32)
                ang = sbuf(ph, "ang", [64, T], F32)
                r1 = sbuf(ph, "r1", [64, T], F32)
                ropec = sbuf(ph, "ropec", [64, 2], F32)
                tr = Res("ropetmp")
                S.dma("sp", posi[:], I["pos" + sfx].partition_broadcast(64), writes=[tr])
                S.dma("sp", ropec[:], I["ropec"], writes=[tr])
                S.op("dve", lambda e: e.tensor_copy(ang[:], posi[:]), reads=[tr], writes=[tr])
                S.op("dve", lambda e: e.tensor_scalar(ang[:], ang[:], ropec[:, 0:1], None, ALU.mult), reads=[tr], writes=[tr])
                ki = sbuf(ph, "ki", [64, T], I32)
                kf = sbuf(ph, "kf", [64, T], F32)

                def reduce_to_pi(shift):
                    S.op("dve", lambda e: e.tensor_scalar(r1[:], ang[:], shift, None, ALU.add), reads=[tr, ropr], writes=[tr])
                    S.op("dve", lambda e: e.tensor_scalar(kf[:], r1[:], 1.0 / (2 * PI), None, ALU.mult), reads=[tr], writes=[tr])
                    S.op("dve", lambda e: e.tensor_copy(ki[:], kf[:]), reads=[tr], writes=[tr])
                    S.op("dve", lambda e: e.tensor_copy(kf[:], ki[:]), reads=[tr], writes=[tr])
                    S.op("dve", lambda e: e.scalar_tensor_tensor(r1[:], kf[:], -2 * PI, r1[:], ALU.mult, ALU.add), reads=[tr], writes=[tr])
                    S.op("dve", lambda e: e.tensor_scalar(kf[:], r1[:], PI, 2 * PI, ALU.is_gt, ALU.mult), reads=[tr], writes=[tr])
                    S.op("dve", lambda e: e.tensor_tensor(r1[:], r1[:], kf[:], ALU.subtract), reads=[tr], writes=[tr])
                    S.op("dve", lambda e: e.tensor_scalar(kf[:], r1[:], -PI, 2 * PI, ALU.is_lt, ALU.mult), reads=[tr], writes=[tr])
                    S.op("dve", lambda e: e.tensor_tensor(r1[:], r1[:], kf[:], ALU.add), reads=[tr], writes=[tr])
                    S.op("dve", lambda e: e.tensor_scalar(r1[:], r1[:], -3.1415925, 3.1415925, ALU.max, ALU.min), reads=[tr], writes=[tr])

                reduce_to_pi(0.0)
                S.op("act", lambda e: e.activation(sn_t[:], r1[:], AF.Sin, scale=ropec[:, 1:2]), reads=[tr], writes=[ropr])
                reduce_to_pi(PI / 2)
                S.op("act", lambda e: e.activation(cs_t[:], r1[:], AF.Sin), reads=[tr], writes=[ropr])
                if "rope" in dbg:
                    dump("cs", cs_t[:], [64, T], F32, [ropr])
                    dump("sn", sn_t[:], [64, T], F32, [ropr])
                S.flush()
            return cs_t, sn_t, ropr

        def ffn_phase(ph, h2, h2r, wg_src, wu_src, wd_src, n_exp, G, gate_fn):
            wgt = [sbuf(ph, "wg%d" % i, [128, 8, FB * 128], BF16) for i in range(2)]
            wut = [sbuf(ph, "wu%d" % i, [128, 8, FB * 128], BF16) for i in range(2)]
            wdt = [sbuf(ph, "wd%d" % i, [128, FB, D], BF16) for i in range(2)]
            wr = [Res("wslot%d" % i) for i in range(2)]
            ablk = sbuf(ph, "ablk", [128, FB, T], BF16)
            ar = [Res("ablk%d" % g) for g in range(NG)]
            sil = [sbuf(ph, "sil%d" % i, [128, GW], BF16) for i in range(4)]
            silr = [Res("sil%d" % i) for i in range(4)]
            a1 = [sbuf(ph, "a1_%d" % i, [128, GW], BF16) for i in range(4)]
            a1r = [Res("a1_%d" % i) for i in range(4)]
            steps = [(e, fb) for e in range(n_exp) for fb in range(NFB)]

            def load(si):
                e, fb = steps[si]
                s = si % 2
                S.dma("pool", wgt[s][:], wg_src(e, fb), writes=[wr[s]])
                S.dma("pool", wut[s][:], wu_src(e, fb), writes=[wr[s]])
                S.dma("pool", wdt[s][:], wd_src(e, fb), writes=[wr[s]])

            load(0)
            cnt = 0
            dcnt = [0]
            for si, (e, fb) in enumerate(steps):
                s = si % 2
                if si + 1 < len(steps):
                    load(si + 1)
                gw = gate_fn(e) if (gate_fn is not None and fb == 0) else (gate_fn.cur if gate_fn is not None else None)
                for g in range(NG):
                    for fi in range(FB):
                        k = cnt % 2
                        cnt += 1
                        bg, bu = k, 2 + k
                        for c in range(8):
                            S.op("pe", lambda e_, c=c, s=s, fi=fi, g=g, bg=bg: e_.matmul(
                                pb[bg][:], wgt[s][:, c, fi * 128:(fi + 1) * 128], h2[:, c, g * GW:(g + 1) * GW], start=(c == 0), stop=(c == 7)),
                                reads=[wr[s], h2r[g]], writes=[pbr[bg]], inc=(c == 7))
                        for c in range(8):
                            S.op("pe", lambda e_, c=c, s=s, fi=fi, g=g, bu=bu: e_.matmul(
                                pb[bu][:], wut[s][:, c, fi * 128:(fi + 1) * 128], h2[:, c, g * GW:(g + 1) * GW], start=(c == 0), stop=(c == 7)),
                                reads=[wr[s], h2r[g]], writes=[pbr[bu]], inc=(c == 7))
                        kk = (cnt - 1) % 4
                        S.op("act", lambda e_, kk=kk, bg=bg: e_.activation(sil[kk][:], pb[bg][:], AF.Silu),
                             reads=[pbr[bg]], writes=[silr[kk]])
                        if gw is None:
                            S.op("dve", lambda e_, kk=kk, bu=bu, fi=fi, g=g: e_.tensor_tensor(
                                ablk[:, fi, g * GW:(g + 1) * GW], sil[kk][:], pb[bu][:], ALU.mult),
                                reads=[silr[kk], pbr[bu]], writes=[ar[g]])
                        else:
                            gwt, gwr = gw
                            S.op("dve", lambda e_, kk=kk, bu=bu: e_.tensor_tensor(a1[kk][:], sil[kk][:], pb[bu][:], ALU.mult),
                                 reads=[silr[kk], pbr[bu]], writes=[a1r[kk]])
                            S.op("dve", lambda e_, kk=kk, fi=fi, g=g, gwt=gwt: e_.tensor_tensor(
                                ablk[:, fi, g * GW:(g + 1) * GW], a1[kk][:], gwt[:, g * GW:(g + 1) * GW], ALU.mult),
                                reads=[a1r[kk], gwr], writes=[ar[g]])
                for g in range(NG):
                    for dc in range(8):
                        bd = 4 + (dcnt[0] % 3)
                        dcnt[0] += 1
                        for fi in range(FB):
                            S.op("pe", lambda e_, fi=fi, s=s, dc=dc, g=g, bd=bd: e_.matmul(
                                pb[bd][:], wdt[s][:, fi, dc * 128:(dc + 1) * 128], ablk[:, fi, g * GW:(g + 1) * GW], start=(fi == 0), stop=(fi == FB - 1)),
                                reads=[wr[s], ar[g]], writes=[pbr[bd]], inc=(fi == FB - 1))
                        S.op("dve", lambda e_, dc=dc, g=g, bd=bd: e_.scalar_tensor_tensor(
                            x[:, dc, g * GW:(g + 1) * GW], pb[bd][:], G(dc), x[:, dc, g * GW:(g + 1) * GW], ALU.mult, ALU.add),
                            reads=[pbr[bd], mres, xr[g]], writes=[xr[g]])

        def layer0_pass(sfx, slot, tctx):
            xsrc = I["xT" + sfx].rearrange("(c p) t -> p c t", p=128)
            for g in range(NG):
                S.dma("sp", x[:, :, g * GW:(g + 1) * GW], xsrc[:, :, g * GW:(g + 1) * GW], writes=[xr[g]])
            with contextlib.ExitStack() as ph:
                w_in = sbuf(ph, "w_in", [128, 8, D], BF16)
                w_grp = sbuf(ph, "w_grp", [128, 4, 2, 256], BF16)
                w_out = sbuf(ph, "w_out", [128, 8, D], BF16)
                wres = Res("poolw")
                S.dma("pool", w_in[:], I["pool_w_in"].rearrange("(c p) f -> p c f", p=128), writes=[wres])
                S.dma("pool", w_grp[:], I["pool_w_grp"].rearrange("g (k p) f -> p g k f", p=128), writes=[wres])
                S.dma("pool", w_out[:], I["pool_w_out"].rearrange("(c p) f -> p c f", p=128), writes=[wres])
                xh = sbuf(ph, "xh", [128, 8, 256], F32)
                xhr = Res("xh")
                S.dma("sp", xh[:], I["xhT" + sfx].rearrange("(c p) t -> p c t", p=128), writes=[xhr])
                hmask = sbuf(ph, "hmask", [128, 256], F32)
                invc = sbuf(ph, "invc", [128, 8, 16], F32)
                S.dma("sp", hmask[:], I["hmask" + sfx], writes=[cres])
                S.dma("sp", invc[:], I["invc" + sfx], writes=[cres])
                UW = 256
                uh = sbuf(ph, "uh", [128, 8, 16, 16], F32)
                uhr = Res("uh")
                t16 = sbuf(ph, "t16", [128, 2, 16], F32)
                t16r = Res("t16")
                sets = []
                for bs in range(2):
                    big = sbuf(ph, "big%d" % bs, [128, 8, 288], F32)
                    ugr = [Res("ug%d_%d" % (bs, c)) for c in range(8)]
                    d_ = dict(
                        big=big, ugr=ugr, ug=big[:].rearrange("p c (t q) -> p c t q", q=144),
                        ns=NormScratch(ph, "p%d" % bs, n=UW, tmp=big, r_tmp=ugr),
                        h=sbuf(ph, "h_p%d" % bs, [128, 8, UW], BF16), hr=Res("h_p%d" % bs),
                        sA=sbuf(ph, "sA%d" % bs, [128, 2, 2, 144], F32), sAr=Res("sA%d" % bs),
                        sB=sbuf(ph, "sB%d" % bs, [128, 2, 2, 144], F32), sBr=Res("sB%d" % bs),
                        pz=sbuf(ph, "pz%d" % bs, [128, 8, UW], BF16), pzr=Res("pz%d" % bs),
                        nbank=(0, 7)[bs],
                    )
                    sets.append(d_)
                bk = [0]

                def nextbank():
                    bk[0] = (bk[0] % 6) + 1
                    return bk[0]

                s0 = sets[0]
                h = s0["h"]
                hr = s0["hr"]
                norm_mod(s0["ns"], lambda c: xh[:, c, :], [xhr], 8, 256, D, A1(0), B1(0), lambda c: h[:, c, :256], [hr], 0)
                uhf = uh[:].rearrange("p c a b -> p c (a b)")
                for oc in range(8):
                    b = nextbank()
                    for c in range(8):
                        S.op("pe", lambda e, c=c, oc=oc, b=b: e.matmul(pb[b][:, :256], w_in[:, c, oc * 128:(oc + 1) * 128], h[:, c, :256], start=(c == 0), stop=(c == 7)),
                             reads=[wres, hr], writes=[pbr[b]], inc=(c == 7))
                    S.op("dve", lambda e, oc=oc, b=b: e.tensor_tensor(uhf[:, oc, :], pb[b][:, :256], hmask[:], ALU.mult),
                         reads=[pbr[b], cres], writes=[uhr])

                def unit(u):
                    st_ = sets[u % 2]
                    h, hr, ug, ugr, pz, pzr, ns = st_["h"], st_["hr"], st_["ug"], st_["ugr"], st_["pz"], st_["pzr"], st_["ns"]
                    zt, ztr = h, hr
                    g = u // 2
                    us = slice(u * UW, (u + 1) * UW)
                    norm_mod(ns, lambda c: x[:, c, us], [xr[g]], 8, UW, D, A1(0), B1(0), lambda c: h[:, c, :], [hr], st_["nbank"], src_all=x[:, :, us])
                    yield
                    for oc in range(8):
                        b = nextbank()
                        for c in range(8):
                            S.op("pe", lambda e, c=c, oc=oc, b=b: e.matmul(pb[b][:, :UW], w_in[:, c, oc * 128:(oc + 1) * 128], h[:, c, :], start=(c == 0), stop=(c == 7)),
                                 reads=[wres, hr], writes=[pbr[b]], inc=(c == 7))
                        S.op("act", lambda e, oc=oc, b=b: e.copy(ug[:, oc, :, 16:144], pb[b][:, :UW].rearrange("p (t q) -> p t q", q=128)),
                             reads=[pbr[b]], writes=[ugr[oc]])
                    S.op("dve", lambda e: e.tensor_copy(ug[:, :, :, 0:16], uh[:, :, u * 2:(u + 1) * 2, :]),
                         reads=[uhr], writes=ugr)
                    yield
                    for wgp in range(4):
                        cs = slice(2 * wgp, 2 * wgp + 2)
                        w = 2 << wgp
                        lo = 0
                        step = 1
                        bufs = [(st_["sA"], st_["sAr"]), (st_["sB"], st_["sBr"])]
                        bi = 0
                        ugp = [ugr[2 * wgp], ugr[2 * wgp + 1]]
                        cur, cur_r = ug[:, cs], ugp
                        while step < w:
                            dst, dst_r = bufs[bi]
                            bi ^= 1
                            S.op("dve", lambda e, cur=cur, dst=dst, lo=lo, step=step: e.tensor_tensor(
                                dst[:, :, :, lo + step:144], cur[:, :, :, lo + step:144], cur[:, :, :, lo:144 - step], ALU.add),
                                reads=cur_r, writes=[dst_r])
                            cur, cur_r = dst[:], [dst_r]
                            lo += step
                            step *= 2
                        S.op("dve", lambda e, cur=cur, cs=cs, w=w: e.scalar_tensor_tensor(
                            pz[:, cs, :].rearrange("p c (t q) -> p c t q", q=128), cur[:, :, :, 16:144], 1.0 / w, ug[:, cs, :, 16:144], ALU.mult, ALU.subtract),
                            reads=cur_r + ugp, writes=[pzr])
                        if u == 0:
                            S.op("dve", lambda e, cur=cur, cs=cs: e.tensor_tensor(t16[:], cur[:, :, 0, 16:32], invc[:, cs, :], ALU.mult),
                                 reads=cur_r + [cres], writes=[t16r])
                            S.op("dve", lambda e, cs=cs: e.tensor_tensor(pz[:, cs, 0:16], t16[:], ug[:, cs, 0, 16:32], ALU.subtract),
                                 reads=[t16r, pzr] + ugp, writes=[pzr])
                    yield
                    for wgp in range(4):
                        for oc2 in range(2):
                            b = nextbank()
                            for k2 in range(2):
                                S.op("pe", lambda e, wgp=wgp, oc2=oc2, k2=k2, b=b: e.matmul(
                                    pb[b][:, :UW], w_grp[:, wgp, k2, oc2 * 128:(oc2 + 1) * 128], pz[:, 2 * wgp + k2, :], start=(k2 == 0), stop=(k2 == 1)),
                                    reads=[wres, pzr], writes=[pbr[b]], inc=(k2 == 1))
                            oc = 2 * wgp + oc2
                            S.op("act", lambda e, oc=oc, b=b: e.activation(zt[:, oc, :], pb[b][:, :UW], AF.Identity, scale=col(vecs, VEC_PSC + oc)),
                                 reads=[pbr[b], cres], writes=[ztr])
                    yield
                    for oc in range(8):
                        b = nextbank()
                        for c in range(8):
                            S.op("pe", lambda e, c=c, oc=oc, b=b: e.matmul(pb[b][:, :UW], w_out[:, c, oc * 128:(oc + 1) * 128], zt[:, c, :], start=(c == 0), stop=(c == 7)),
                                 reads=[wres, ztr], writes=[pbr[b]], inc=(c == 7))
                        S.op("dve", lambda e, oc=oc, b=b: e.scalar_tensor_tensor(
                            x[:, oc, us], pb[b][:, :UW], G1(0)(oc), x[:, oc, us], ALU.mult, ALU.add),
                            reads=[pbr[b], mres, xr[g]], writes=[xr[g]])

                run_pipelined([unit(u) for u in range(8)], 2)
                if "x_pool" in dbg:
                    dump("x_pool", x[:], [128, 8, T], F32, xr)
                S.flush()

            with contextlib.ExitStack() as ph:
                h2 = sbuf(ph, "h2", [128, 8, T], BF16)
                h2r = [Res("h2_%d" % g) for g in range(NG)]
                with contextlib.ExitStack() as ph2:
                    ns = NormScratch(ph2, "f")
                    for g in range(NG):
                        norm_mod(ns, lambda c, g=g: x[:, c, g * GW:(g + 1) * GW], [xr[g]], 8, GW, D, A2(0), B2(0),
                                 lambda c, g=g: h2[:, c, g * GW:(g + 1) * GW], [h2r[g]], 7, src_all=x[:, :, g * GW:(g + 1) * GW])
                    S.flush()
                wg_src = lambda e, fb: I["ffn_w_gate"][e][:, fb * 512:(fb + 1) * 512].rearrange("(c p) f -> p c f", p=128)
                wu_src = lambda e, fb: I["ffn_w_up"][e][:, fb * 512:(fb + 1) * 512].rearrange("(c p) f -> p c f", p=128)
                wd_src = lambda e, fb: I["ffn_w_down"][e][fb * 512:(fb + 1) * 512, :].rearrange("(fi p) d -> p fi d", p=128)
                ffn_phase(ph, h2, h2r, wg_src, wu_src, wd_src, 1, G2(0), None)
                if "x_l0" in dbg:
                    dump("x_l0", x[:], [128, 8, T], F32, xr)
                S.flush()

            cs_t, sn_t, ropr = make_tables(tctx, sfx)
            with contextlib.ExitStack() as ph:
                UW = 256
                w_dkv = sbuf(ph, "w_dkv", [128, 8, 256], BF16)
                w_kr2 = sbuf(ph, "w_kr2", [128, 8, 128], BF16)
                wres = Res("kvw")
                S.dma("pool", w_dkv[:], I["w_dkv"].rearrange("(c p) f -> p c f", p=128), writes=[wres])
                S.dma("pool", w_kr2[:], I["w_kr2"].rearrange("(c p) f -> p c f", p=128), writes=[wres])
                Bkv = lambda c: col(modc, 96 + c)
                Akv = lambda c: col(AB, 32 + c)
                lsets = []
                for bs in range(2):
                    lsets.append(dict(
                        ns=NormScratch(ph, "k%d" % bs, n=UW), ns2=NormScratch(ph, "k2%d" % bs, nch=2, n=UW),
                        hk=sbuf(ph, "hk%d" % bs, [128, 8, UW], BF16), hkr=Res("hk%d" % bs),
                        t1=sbuf(ph, "t1%d" % bs, [64, UW], F32), t2=sbuf(ph, "t2%d" % bs, [64, UW], F32),
                        t1r=Res("t1%d" % bs), t2r=Res("t2%d" % bs),
                        latg=sbuf(ph, "latg%d" % bs, [128, 2, UW], BF16), latgr=sbuf(ph, "latgr%d" % bs, [64, UW], BF16),
                        lgr=Res("latg%d" % bs), base=4 * bs))

                def lat_unit(u):
                    st_ = lsets[u % 2]
                    ns, ns2, hk, hkr, t1, t2, t1r, t2r = st_["ns"], st_["ns2"], st_["hk"], st_["hkr"], st_["t1"], st_["t2"], st_["t1r"], st_["t2r"]
                    latg, latgr, lgr, base = st_["latg"], st_["latgr"], st_["lgr"], st_["base"]
                    g = u // 2
                    us = slice(u * UW, (u + 1) * UW)
                    norm_mod(ns, lambda c: x[:, c, us], [xr[g]], 8, UW, D, Akv, Bkv, lambda c: hk[:, c, :], [hkr], base, src_all=x[:, :, us])
                    yield
                    for oc in range(2):
                        for c in range(8):
                            S.op("pe", lambda e, c=c, oc=oc: e.matmul(pb[base + 1][:, oc * UW:(oc + 1) * UW], w_dkv[:, c, oc * 128:(oc + 1) * 128], hk[:, c, :], start=(c == 0), stop=(c == 7)),
                                 reads=[wres, hkr], writes=[pbr[base + 1]], inc=(c == 7))
                    for sw in range(2):
                        for c in range(8):
                            S.op("pe", lambda e, c=c, sw=sw: e.matmul(pb[base + 2][0:64, sw * UW:(sw + 1) * UW], w_kr2[:, c, sw * 64:(sw + 1) * 64], hk[:, c, :], start=(c == 0), stop=(c == 7)),
                                 reads=[wres, hkr], writes=[pbr[base + 2]], inc=(c == 7))
                    yield
                    norm_mod(ns2, lambda c: pb[base + 1][:, c * UW:(c + 1) * UW], [pbr[base + 1]], 2, UW, 256.0, lambda c: col(vecs, VEC_CKVG + c), None,
                             lambda c: latg[:, c, :], [lgr], base + 3)
                    S.op("dve", lambda e: e.tensor_tensor(t1[:], pb[base + 2][0:64, 0:UW], cs_t[:, us], ALU.mult), reads=[pbr[base + 2], ropr], writes=[t1r])
                    S.op("dve", lambda e: e.tensor_tensor(t2[:], pb[base + 2][0:64, UW:2 * UW], sn_t[:, us], ALU.mult), reads=[pbr[base + 2], ropr], writes=[t2r])
                    S.op("dve", lambda e: e.tensor_tensor(latgr[:], t1[:], t2[:], ALU.add), reads=[t1r, t2r], writes=[lgr])
                    S.op("dve", lambda e: e.tensor_copy(Lc5[:, :, 2 * u:2 * u + 2, slot, :], latg[:].rearrange("p c (t q) -> p c t q", q=128)),
                         reads=[lgr], writes=[Lres])
                    S.op("dve", lambda e: e.tensor_copy(Lr4[:, 2 * u:2 * u + 2, slot, :], latgr[:].rearrange("p (t q) -> p t q", q=128)),
                         reads=[lgr], writes=[Lres])

                run_pipelined([lat_unit(u) for u in range(8)], 2)
                S.flush()
            return cs_t, sn_t, ropr

        with contextlib.ExitStack() as tmpctx:
            layer0_pass("_p", 1, tmpctx)
        tctx = st.enter_context(contextlib.ExitStack())
        cs_t, sn_t, ropr = layer0_pass("", 0, tctx)

        if doB:
            with contextlib.ExitStack() as ph:
                cq = sbuf(ph, "cq", [128, 3, T], BF16)
                cqr = Res("cq")
                w_uk = sbuf(ph, "w_uk", [128, 2, D], BF16)
                w_uv = sbuf(ph, "w_uv", [128, 2, D], BF16)
                w_uqn = sbuf(ph, "w_uqn", [128, 3, D], BF16)
                w_uqr = sbuf(ph, "w_uqr", [128, 3, D], BF16)
                w_o = sbuf(ph, "w_o", [128, 8, D], BF16)
                amask = sbuf(ph, "amask", [128, 4, 128], BF16)
                wres = Res("attw")
                S.dma("pool", w_uk[:], I["w_uk"].rearrange("(c p) f -> p c f", p=128), writes=[wres])
                S.dma("pool", w_uv[:], I["w_uv"].rearrange("(c p) f -> p c f", p=128), writes=[wres])
                S.dma("pool", w_uqn[:], I["w_uq_n"].rearrange("(c p) f -> p c f", p=128), writes=[wres])
                S.dma("pool", w_uqr[:], I["w_uq_r"].rearrange("(c p) f -> p c f", p=128), writes=[wres])
                S.dma("pool", w_o[:], I["w_o"].rearrange("(c p) f -> p c f", p=128), writes=[wres])
                S.dma("pool", amask[:], I["amask"], writes=[wres])
                with contextlib.ExitStack() as ph2:
                    UW = 256
                    w_dq = sbuf(ph2, "w_dq", [128, 8, 384], BF16)
                    qwres = Res("wdq")
                    S.dma("pool", w_dq[:], I["w_dq"].rearrange("(c p) f -> p c f", p=128), writes=[qwres])
                    qsets = []
                    for bs in range(2):
                        qsets.append(dict(
                            ns=NormScratch(ph2, "a%d" % bs, n=UW), ns3=NormScratch(ph2, "a3%d" % bs, nch=3, n=UW),
                            hq=sbuf(ph2, "hq%d" % bs, [128, 8, UW], BF16), hqr=Res("hq%d" % bs), base=4 * bs))

                    def cq_unit(u):
                        st_ = qsets[u % 2]
                        ns, ns3, hq, hqr, base = st_["ns"], st_["ns3"], st_["hq"], st_["hqr"], st_["base"]
                        g = u // 2
                        us = slice(u * UW, (u + 1) * UW)
                        norm_mod(ns, lambda c: x[:, c, us], [xr[g]], 8, UW, D, A1(1), B1(1), lambda c: hq[:, c, :], [hqr], base, src_all=x[:, :, us])
                        yield
                        for oc in range(3):
                            for c in range(8):
                                S.op("pe", lambda e, c=c, oc=oc: e.matmul(pb[base + 1 + oc // 2][:, (oc % 2) * UW:(oc % 2 + 1) * UW], w_dq[:, c, oc * 128:(oc + 1) * 128], hq[:, c, :],
                                                                         start=(c == 0), stop=(c == 7)),
                                     reads=[qwres, hqr], writes=[pbr[base + 1 + oc // 2]], inc=(c == 7))
                        yield
                        norm_mod(ns3, lambda c: pb[base + 1 + c // 2][:, (c % 2) * UW:(c % 2 + 1) * UW], [pbr[base + 1], pbr[base + 2]], 3, UW, 384.0,
                                 lambda c: col(vecs, VEC_QG + c), None, lambda c: cq[:, c, us], [cqr], base + 3)

                    run_pipelined([cq_unit(u) for u in range(8)], 2)
                    S.flush()
                KT = sbuf(ph, "KT", [128, 2 * T], BF16)
                KTr = Res("KT")
                Vh = sbuf(ph, "Vh", [128, 32, 128], BF16)
                Vhr = Res("Vh")
                QT = sbuf(ph, "QT", [128, T], BF16)
                QTr = Res("QT")
                QR = sbuf(ph, "QR", [64, T], BF16)
                QRr = Res("QR")
                t1 = sbuf(ph, "qt1", [64, GW], F32)
                t2 = sbuf(ph, "qt2", [64, GW], F32)
                t1r, t2r = Res("qt1"), Res("qt2")
                oT2 = sbuf(ph, "oT", [128, 2, T], BF16)
                oTr2 = [[Res("oT%d_%d" % (i, g)) for g in range(NG)] for i in range(2)]
                pT = [sbuf(ph, "pT%d" % i, [128, GW], BF16) for i in range(4)]
                pTr = [Res("pT%d" % i) for i in range(4)]
                rcp = sbuf(ph, "rcp", [128, GW], F32)
                rcpr = Res("rcp")
                pcnt = 0
                for hd in range(8):
                    hs = slice(hd * 128, (hd + 1) * 128)
                    oT = oT2[:, hd % 2, :]
                    oTr = oTr2[hd % 2]
                    for kg in range(8):
                        b = kg % 2
                        ks = slice(kg * GW, (kg + 1) * GW)
                        for c in range(2):
                            S.op("pe", lambda e, c=c, ks=ks, b=b, hs=hs: e.matmul(pb[b][:], w_uk[:, c, hs], Lc[:, c, ks], start=(c == 0), stop=(c == 1)),
                                 reads=[wres, Lres], writes=[pbr[b]], inc=(c == 1))
                        S.op("act", lambda e, ks=ks, b=b: e.copy(KT[:, ks], pb[b][:]), reads=[pbr[b]], writes=[KTr])
                    for kq in range(8):
                        b = 2 + kq % 2
                        for ti in range(4):
                            kt = kq * 4 + ti
                            for c in range(2):
                                S.op("pe", lambda e, c=c, kt=kt, ti=ti, b=b, hs=hs: e.matmul(
                                    pb[b][:, ti * 128:(ti + 1) * 128], Lc[:, c, kt * 128:(kt + 1) * 128], w_uv[:, c, hs], start=(c == 0), stop=(c == 1)),
                                    reads=[wres, Lres], writes=[pbr[b]], inc=(c == 1 and ti == 3))
                        S.op("dve", lambda e, kq=kq, b=b: e.tensor_copy(Vh[:, kq * 4:(kq + 1) * 4, :], pb[b][:].rearrange("p (t q) -> p t q", q=128)),
                             reads=[pbr[b]], writes=[Vhr])
                    for g in range(NG):
                        gs = slice(g * GW, (g + 1) * GW)
                        b = 4
                        for c in range(3):
                            S.op("pe", lambda e, c=c, gs=gs, hs=hs: e.matmul(pb[4][:], w_uqn[:, c, hs], cq[:, c, gs], start=(c == 0), stop=(c == 2)),
                                 reads=[wres, cqr], writes=[pbr[4]], inc=(c == 2))
                        S.op("act", lambda e, gs=gs: e.copy(QT[:, gs], pb[4][:]), reads=[pbr[4]], writes=[QTr])
                        for sw in range(2):
                            for c in range(3):
                                S.op("pe", lambda e, c=c, gs=gs, sw=sw, hd=hd: e.matmul(
                                    pb[5 + sw][0:64, :], w_uqr[:, c, hd * 128 + sw * 64:hd * 128 + (sw + 1) * 64], cq[:, c, gs], start=(c == 0), stop=(c == 2)),
                                    reads=[wres, cqr], writes=[pbr[5 + sw]], inc=(c == 2))
                        S.op("dve", lambda e, gs=gs: e.tensor_tensor(t1[:], pb[5][0:64, :], cs_t[:, gs], ALU.mult), reads=[pbr[5], ropr], writes=[t1r])
                        S.op("dve", lambda e, gs=gs: e.tensor_tensor(t2[:], pb[6][0:64, :], sn_t[:, gs], ALU.mult), reads=[pbr[6], ropr], writes=[t2r])
                        S.op("dve", lambda e, gs=gs: e.tensor_tensor(QR[:, gs], t1[:], t2[:], ALU.add), reads=[t1r, t2r], writes=[QRr])
                    for m in range(NG):
                        j0 = 4 * m
                        nkt = 8 * m + 8
                        bo, br_ = (2, 3) if m % 2 == 0 else (4, 5)
                        info = {}

                        def emit_S(kt):
                            nonlocal pcnt
                            jmin = max(j0, kt // 2)
                            c0 = (jmin - j0) * 128
                            n = GW - c0
                            qs = slice(m * GW + c0, (m + 1) * GW)
                            bs = kt % 2
                            S.op("pe", lambda e, kt=kt, qs=qs, bs=bs, n=n: e.matmul(pb[bs][:, :n], KT[:, kt * 128:(kt + 1) * 128], QT[:, qs], start=True, stop=False),
                                 reads=[KTr, QTr], writes=[pbr[bs]], inc=False)
                            S.op("pe", lambda e, kt=kt, qs=qs, bs=bs, n=n: e.matmul(pb[bs][:, :n], Lr[:, kt * 128:(kt + 1) * 128], QR[:, qs], start=False, stop=True),
                                 reads=[Lres, QRr], writes=[pbr[bs]])
                            pi_ = pcnt % 4
                            pcnt += 1
                            S.op("act", lambda e, pi_=pi_, bs=bs, n=n: e.activation(pT[pi_][:, :n], pb[bs][:, :n], AF.Exp, scale=ATTN_SCALE),
                                 reads=[pbr[bs]], writes=[pTr[pi_]])
                            if kt // 2 >= j0:
                                mi = 2 * ((kt // 2) % 2) + (kt % 2)
                                S.op("dve", lambda e, pi_=pi_, mi=mi: e.tensor_tensor(pT[pi_][:, 0:128], pT[pi_][:, 0:128], amask[:, mi, :], ALU.mult),
                                     reads=[pTr[pi_], wres], writes=[pTr[pi_]])
                            info[kt] = (pi_, c0, n)

                        def emit_PV(kt):
                            pi_, c0, n = info[kt]
                            S.op("pe", lambda e, kt=kt, pi_=pi_, c0=c0, n=n, bo=bo, nkt=nkt: e.matmul(pb[bo][:, c0:GW], Vh[:, kt, :], pT[pi_][:, :n], start=(kt == 0), stop=(kt == nkt - 1)),
                                 reads=[Vhr, pTr[pi_]], writes=[pbr[bo]], inc=False)
                            S.op("pe", lambda e, kt=kt, pi_=pi_, c0=c0, n=n, br_=br_, nkt=nkt: e.matmul(pb[br_][:, c0:GW], ones[:], pT[pi_][:, :n], start=(kt == 0), stop=(kt == nkt - 1)),
                                 reads=[cres, pTr[pi_]], writes=[pbr[br_]])

                        emit_S(0)
                        for kt in range(nkt):
                            if kt + 1 < nkt:
                                emit_S(kt + 1)
                            emit_PV(kt)
                        S.op("act", lambda e, br_=br_: e.activation(rcp[:], pb[br_][:], AF.Ln), reads=[pbr[br_]], writes=[rcpr])
                        S.op("act", lambda e: e.activation(rcp[:], rcp[:], AF.Exp, scale=-1.0), reads=[rcpr], writes=[rcpr])
                        S.op("dve", lambda e, m=m, bo=bo, oT=oT: e.tensor_tensor(oT[:, m * GW:(m + 1) * GW], pb[bo][:], rcp[:], ALU.mult),
                             reads=[pbr[bo], rcpr], writes=[oTr[m]])
                    if "oT0" in dbg and hd == 0:
                        dump("oT0", oT[:], [128, T], BF16, oTr)
                        dump("QT0", QT[:], [128, T], BF16, [QTr])
                        dump("QR0", QR[:], [64, T], BF16, [QRr])
                        dump("KT0", KT[:], [128, 2 * T], BF16, [KTr])
                    if hd % 2 == 0:
                        continue
                    for g in range(NG):
                        gs = slice(g * GW, (g + 1) * GW)
                        for dc in range(8):
                            b = 6 + dc % 2
                            for hh in range(2):
                                S.op("pe", lambda e, dc=dc, gs=gs, b=b, hd=hd, hh=hh: e.matmul(
                                    pb[b][:], w_o[:, hd - 1 + hh, dc * 128:(dc + 1) * 128], oT2[:, hh, gs], start=(hh == 0), stop=(hh == 1)),
                                    reads=[wres, oTr2[hh][g]], writes=[pbr[b]], inc=(hh == 1))
                            S.op("dve", lambda e, dc=dc, gs=gs, b=b: e.scalar_tensor_tensor(x[:, dc, gs], pb[b][:], G1(1)(dc), x[:, dc, gs], ALU.mult, ALU.add),
                                 reads=[pbr[b], mres, xr[g]], writes=[xr[g]])
                if "x_att" in dbg:
                    dump("x_att", x[:], [128, 8, T], F32, xr)
                S.flush()
            tctx.close()
            Lctx.close()

            with contextlib.ExitStack() as ph:
                h2 = sbuf(ph, "h2m", [128, 8, T], BF16)
                h2r = [Res("h2m_%d" % g) for g in range(NG)]
                gT = sbuf(ph, "gT", [8, T], F32)
                gTr = Res("gT")
                esel = sbuf(ph, "esel", [8, 8, 128], F32)
                S.dma("sp", esel[:], I["esel"], writes=[cres])
                with contextlib.ExitStack() as ph2:
                    ns = NormScratch(ph2, "m")
                    h32 = sbuf(ph2, "h32", [128, 8, GW], F32)
                    h32r = Res("h32")
                    rw = sbuf(ph2, "rw", [128, 8, 8], F32)
                    rwr = Res("rw")
                    S.dma("sp", rw[:], I["router_w"].rearrange("(c p) e -> p c e", p=128), writes=[rwr])
                    L = sbuf(ph2, "L", [128, 16, 8], F32)
                    L2 = sbuf(ph2, "L2", [128, 16, 8], F32)
                    eq1 = sbuf(ph2, "eq1", [128, 16, 8], F32)
                    eq2 = sbuf(ph2, "eq2", [128, 16, 8], F32)
                    m1 = sbuf(ph2, "m1", [128, 16], F32)
                    m2 = sbuf(ph2, "m2", [128, 16], F32)
                    w1 = sbuf(ph2, "w1", [128, 16], F32)
                    w2 = sbuf(ph2, "w2", [128, 16], F32)
                    gates = sbuf(ph2, "gates", [128, 16, 8], F32)
                    rr = Res("router")
                    lview = pb[7][:, 0:128].rearrange("p (a b) -> p a b", b=8)
                    for g in range(NG):
                        gs = slice(g * GW, (g + 1) * GW)
                        norm_mod(ns, lambda c, gs=gs: x[:, c, gs], [xr[g]], 8, GW, D, A2(1), B2(1), lambda c: h32[:, c, :], [h32r], 6, src_all=x[:, :, gs])
                        S.op("act", lambda e, gs=gs: e.copy(h2[:, :, gs], h32[:]), reads=[h32r], writes=[h2r[g]])
                        for ti in range(4):
                            tt = g * 4 + ti
                            for c in range(8):
                                S.op("pe", lambda e, c=c, ti=ti, tt=tt: e.matmul(lview[:, tt, :], h32[:, c, ti * 128:(ti + 1) * 128], rw[:, c, :], start=(c == 0), stop=(c == 7)),
                                     reads=[h32r, rwr], writes=[pbr[7]], inc=(c == 7))
                    S.op("dve", lambda e: e.tensor_copy(L[:], lview), reads=[pbr[7]], writes=[rr])
                    bc = lambda t: t[:].unsqueeze(2).to_broadcast([128, 16, 8])
                    S.op("dve", lambda e: e.tensor_reduce(m1[:], L[:], AX.X, ALU.max), reads=[rr], writes=[rr])
                    S.op("dve", lambda e: e.tensor_tensor(eq1[:], L[:], bc(m1), ALU.is_equal), reads=[rr], writes=[rr])
                    S.op("dve", lambda e: e.scalar_tensor_tensor(L2[:], eq1[:], -1e30, L[:], ALU.mult, ALU.add), reads=[rr], writes=[rr])
                    S.op("dve", lambda e: e.tensor_reduce(m2[:], L2[:], AX.X, ALU.max), reads=[rr], writes=[rr])
                    S.op("dve", lambda e: e.tensor_tensor(eq2[:], L2[:], bc(m2), ALU.is_equal), reads=[rr], writes=[rr])
                    S.op("dve", lambda e: e.tensor_tensor(w2[:], m2[:], m1[:], ALU.subtract), reads=[rr], writes=[rr])
                    S.op("act", lambda e: e.activation(w2[:], w2[:], AF.Exp), reads=[rr], writes=[rr])
                    S.op("dve", lambda e: e.tensor_scalar(w1[:], w2[:], 1.0, None, ALU.add), reads=[rr], writes=[rr])
                    S.op("dve", lambda e: e.reciprocal(w1[:], w1[:]), reads=[rr], writes=[rr])
                    S.op("dve", lambda e: e.tensor_tensor(w2[:], w2[:], w1[:], ALU.mult), reads=[rr], writes=[rr])
                    S.op("dve", lambda e: e.tensor_tensor(gates[:], eq1[:], bc(w1), ALU.mult), reads=[rr], writes=[rr])
                    S.op("dve", lambda e: e.tensor_tensor(eq2[:], eq2[:], bc(w2), ALU.mult), reads=[rr], writes=[rr])
                    S.op("dve", lambda e: e.tensor_tensor(gates[:], gates[:], eq2[:], ALU.add), reads=[rr], writes=[rr])
                    for tt in range(16):
                        b = 4 + (tt // 4) % 2
                        S.op("pe", lambda e, tt=tt, b=b: e.transpose(pb[b][0:8, (tt % 4) * 128:(tt % 4 + 1) * 128], gates[:, tt, :], ident[:]),
                             reads=[rr, cres], writes=[pbr[b]], inc=(tt % 4 == 3))
                        if tt % 4 == 3:
                            g = tt // 4
                            S.op("dve", lambda e, g=g, b=b: e.tensor_copy(gT[:, g * GW:(g + 1) * GW], pb[b][0:8, :]), reads=[pbr[b]], writes=[gTr])
                    if "gT" in dbg:
                        dump("gT", gT[:], [8, T], F32, [gTr])
                    S.flush()
                gwb = [sbuf(ph, "gwb%d" % i, [128, T], BF16) for i in range(2)]
                gwbr = [Res("gwb%d" % i) for i in range(2)]

                class GateFn:
                    cur = None

                    def __call__(self, e):
                        i = e % 2
                        for g in range(NG):
                            b = 7
                            S.op("pe", lambda e_, g=g, b=b, e=e: e_.matmul(pb[b][:], esel[:, e, :], gT[:, g * GW:(g + 1) * GW], start=True, stop=True),
                                 reads=[cres, gTr], writes=[pbr[b]])
                            S.op("act", lambda e_, g=g, b=b, i=i: e_.copy(gwb[i][:, g * GW:(g + 1) * GW], pb[b][:]),
                                 reads=[pbr[b]], writes=[gwbr[i]])
                        self.cur = (gwb[i], gwbr[i])
                        return self.cur

                wg_src = lambda e, fb: I["moe_w_gate"][e][:, fb * 512:(fb + 1) * 512].rearrange("(c p) f -> p c f", p=128)
                wu_src = lambda e, fb: I["moe_w_up"][e][:, fb * 512:(fb + 1) * 512].rearrange("(c p) f -> p c f", p=128)
                wd_src = lambda e, fb: I["moe_w_down"][e][fb * 512:(fb + 1) * 512, :].rearrange("(fi p) d -> p fi d", p=128)
                ffn_phase(ph, h2, h2r, wg_src, wu_src, wd_src, NE, G2(1), GateFn())
                if "x_moe" in dbg:
                    dump("x_moe", x[:], [128, 8, T], F32, xr)
                S.flush()

            with contextlib.ExitStack() as ph:
                ns = NormScratch(ph, "o")
                ob = [sbuf(ph, "ob%d" % i, [128, 8, GW], F32) for i in range(2)]
                obr = [Res("ob%d" % i) for i in range(2)]
                odst = O["outT"].rearrange("(c p) t -> p c t", p=128)
                for g in range(NG):
                    gs = slice(g * GW, (g + 1) * GW)
                    i = g % 2
                    norm_mod(ns, lambda c, gs=gs: x[:, c, gs], [xr[g]], 8, GW, D, lambda c: col(vecs, VEC_FING + c), None,
                             lambda c, i=i: ob[i][:, c, :], [obr[i]], g % 2)
                    S.dma("sp", odst[:, :, gs], ob[i][:], reads=[obr[i]])
                S.flush()
    return nc, dbg_out


def tile_map(half):
    if half == 0:
        return [2 * j + (j % 2) for j in range(NT)]
    return [2 * j + 1 - (j % 2) for j in range(NT)]


def _common_inputs(inp):
    f32 = np.float32
    vecs = np.zeros((128, NVEC), f32)

    def colv(v):
        return np.asarray(v, f32).reshape(-1, 128).T

    vecs[:, 0:8] = colv(inp["mix_norm_g"][0])
    vecs[:, 8:16] = colv(inp["mix_norm_g"][1])
    vecs[:, 16:24] = colv(inp["ffn_norm_g"][0])
    vecs[:, 24:32] = colv(inp["ffn_norm_g"][1])
    vecs[:, 32:40] = colv(inp["kv_norm_g"])
    vecs[:, 40:48] = colv(inp["pool_scale"][0])
    vecs[:, 48:56] = colv(inp["final_norm_g"])
    vecs[:, 56:58] = colv(inp["ckv_norm_g"])
    vecs[:, 58:61] = colv(inp["q_norm_g"][0])
    inv_freq = (10000.0 ** (-(np.arange(0, 64, 2, dtype=f32)) / 64)).astype(f32)
    ropec = np.zeros((64, 2), f32)
    ropec[:, 0] = np.concatenate([inv_freq, inv_freq])
    ropec[:32, 1] = -1.0
    ropec[32:, 1] = 1.0
    perm = np.concatenate([np.arange(32, 64), np.arange(0, 32)])
    w_kr = np.asarray(inp["w_kr"], f32)
    w_kr2 = np.ascontiguousarray(np.concatenate([w_kr, w_kr[:, perm]], axis=1))
    w_uq = np.asarray(inp["w_uq"][0], f32).reshape(384, 8, 192)
    w_uq_n = np.ascontiguousarray(w_uq[:, :, :128].reshape(384, D))
    rp = w_uq[:, :, 128:]
    w_uq_r = np.ascontiguousarray(np.concatenate([rp, rp[:, :, perm]], axis=2).reshape(384, D))
    esel = np.zeros((8, 8, 128), f32)
    for e in range(8):
        esel[e, e, :] = 1.0
    return dict(vecs=vecs, ropec=ropec, w_kr2=w_kr2, w_uq_n=w_uq_n, w_uq_r=w_uq_r, esel=esel,
                ident=np.eye(128, dtype=f32))


def _token_inputs(inp, b, half, sfx):
    f32 = np.float32
    tm = tile_map(half)
    d = {}
    xb = np.asarray(inp["x"][b], f32)
    xt = xb.reshape(32, 128, D)[tm].reshape(T, D)
    d["xT" + sfx] = np.ascontiguousarray(xt.T)
    halo = np.zeros((NT, 16, D), f32)
    hmask = np.ones((128, NT, 16), f32)
    for j, gt in enumerate(tm):
        if gt == 0:
            hmask[:, j, :] = 0.0
        else:
            halo[j] = xb[gt * 128 - 16:gt * 128]
    d["xhT" + sfx] = np.ascontiguousarray(halo.reshape(256, D).T)
    d["hmask" + sfx] = hmask.reshape(128, 256)
    invc = np.zeros((128, 8, 16), f32)
    for c in range(8):
        w = 2 << (c // 2)
        if tm[0] == 0:
            invc[:, c, :] = 1.0 / np.minimum(np.arange(1, 17), w).astype(f32)
        else:
            invc[:, c, :] = 1.0 / w
    d["invc" + sfx] = invc
    d["pos" + sfx] = np.asarray(inp["positions"][b]).reshape(32, 128)[tm].reshape(1, T).astype(np.int32)
    return d


def _core_inputs(inp, com, shared, tok, b, half):
    f32 = np.float32
    tm = tile_map(half)
    d = dict(shared)
    d.update(tok[(b, half, "")])
    d.update(tok[(b, 1 - half, "_p")])
    d["cT"] = np.ascontiguousarray(np.asarray(inp["c"][b], f32).reshape(8, 128).T)
    tri = (np.arange(128)[:, None] <= np.arange(128)[None, :]).astype(f32)
    am = np.zeros((128, 4, 128), f32)
    for par in range(2):
        gt_par = tm[par] % 2
        am[:, 2 * par + 0, :] = tri
        am[:, 2 * par + 1, :] = 1.0 if gt_par == 1 else 0.0
    d["amask"] = am
    return d


def _shared_inputs(inp, com):
    f32 = np.float32
    d = {}
    d["vecs"] = com["vecs"]
    d["ident"] = com["ident"]
    d["ropec"] = com["ropec"]
    d["esel"] = com["esel"]
    d["w_kr2"] = com["w_kr2"]
    d["w_uq_n"] = com["w_uq_n"]
    d["w_uq_r"] = com["w_uq_r"]
    d["ada_w"] = np.asarray(inp["ada_w"], f32)
    d["ada_b"] = np.asarray(inp["ada_b"], f32).reshape(1, 12 * D)
    d["pool_w_in"] = np.asarray(inp["pool_w_in"][0], f32)
    d["pool_w_grp"] = np.asarray(inp["pool_w_grp"][0], f32)
    d["pool_w_out"] = np.asarray(inp["pool_w_out"][0], f32)
    d["ffn_w_gate"] = np.asarray(inp["ffn_w_gate"], f32)
    d["ffn_w_up"] = np.asarray(inp["ffn_w_up"], f32)
    d["ffn_w_down"] = np.asarray(inp["ffn_w_down"], f32)
    d["kv_ada_w"] = np.asarray(inp["kv_ada_w"], f32)
    d["kv_ada_b"] = np.asarray(inp["kv_ada_b"], f32).reshape(1, 2 * D)
    d["w_dkv"] = np.asarray(inp["w_dkv"], f32)
    d["w_uk"] = np.asarray(inp["w_uk"], f32)
    d["w_uv"] = np.asarray(inp["w_uv"], f32)
    d["w_dq"] = np.asarray(inp["w_dq"][0], f32)
    d["w_o"] = np.asarray(inp["w_o"][0], f32)
    d["router_w"] = np.asarray(inp["router_w"][0], f32)
    d["moe_w_gate"] = np.asarray(inp["moe_w_gate"][0], f32)
    d["moe_w_up"] = np.asarray(inp["moe_w_up"][0], f32)
    d["moe_w_down"] = np.asarray(inp["moe_w_down"][0], f32)
    return d


_PROGS = {}


def _prog(mode="AB"):
    if mode not in _PROGS:
        _PROGS[mode] = build(mode)[0]
    return _PROGS[mode]


def make_in_maps(inp):
    com = _common_inputs(inp)
    shared = _shared_inputs(inp, com)
    cores = [(b, h) for b in range(4) for h in range(2)]
    tok = {}
    for (b, h) in cores:
        for sfx in ("", "_p"):
            tok[(b, h, sfx)] = _token_inputs(inp, b, h, sfx)
    return cores, [_core_inputs(inp, com, shared, tok, b, h) for (b, h) in cores]


def kernel(**inp):
    cores, maps = make_in_maps(inp)
    res = run_bass_kernel_spmd(_prog(), maps, core_ids=list(range(8))).results
    out = np.zeros((4, 32, 128, D), np.float32)
    for ci, (b, h) in enumerate(cores):
        tm = tile_map(h)
        out[b, tm] = np.asarray(res[ci]["outT"]).T.reshape(NT, 128, D)
    return out.reshape(4, 4096, D)
```
